# Optimizing a Trainium2 kernel written in Bass

```python
import jax, jax.numpy as jnp
from jax import lax
import numpy as np

D_MODEL = 1024
BATCH = 16
SEQ = 2048
DEPTH = 4

N_MIXERS = 2
N_A = (DEPTH + 1) // 2
N_B = DEPTH // 2
EPS = 1e-6

M_HEADS = 8
M_DV = D_MODEL // M_HEADS
M_DK = M_DV // 2
M_CHUNK = 128
GATE_CAP = 15.0
A_IN = 2 * M_HEADS * M_DK + 2 * D_MODEL + 2 * M_HEADS

G_CHUNK = 128
G_FFN = 4 * D_MODEL
G_HALF = G_FFN // 2
G_GROUP = 128
G_GROUPS = G_HALF // G_GROUP

FFN_HIDDEN = 4 * D_MODEL

kernel_name = "hybrid_mlstm_sgu_adaln_trunk"


def rmsnorm(x, g):
    xf = x.astype(jnp.float32)
    y = xf * lax.rsqrt(jnp.mean(xf * xf, axis=-1, keepdims=True) + EPS) * g.astype(jnp.float32)
    return y.astype(x.dtype)


def layernorm(x, g, b):
    xf = x.astype(jnp.float32)
    mu = jnp.mean(xf, axis=-1, keepdims=True)
    var = jnp.mean(jnp.square(xf - mu), axis=-1, keepdims=True)
    y = (xf - mu) * lax.rsqrt(var + EPS) * g.astype(jnp.float32) + b.astype(jnp.float32)
    return y.astype(x.dtype)


def _to_chunks(t, nc, d):
    bsz = t.shape[0]
    return t.reshape(bsz, nc, M_CHUNK, M_HEADS, d).transpose(0, 3, 1, 2, 4).astype(jnp.float32)


def mlstm_mixer(h, w_in, b_if, hnorm_g, w_out):
    bsz, s, _ = h.shape
    nc = s // M_CHUNK
    hk = M_HEADS * M_DK
    p = h @ w_in
    q, k, v, o, ig, fg = jnp.split(
        p, [hk, 2 * hk, 2 * hk + D_MODEL, 2 * hk + 2 * D_MODEL, 2 * hk + 2 * D_MODEL + M_HEADS], axis=-1)
    q = _to_chunks(q, nc, M_DK)
    k = _to_chunks(k, nc, M_DK) * (M_DK ** -0.5)
    v = _to_chunks(v, nc, M_DV)
    b_if = b_if.astype(jnp.float32)
    ig = ig.astype(jnp.float32) + b_if[:M_HEADS]
    fg = fg.astype(jnp.float32) + b_if[M_HEADS:]
    ig = GATE_CAP * jnp.tanh(ig / GATE_CAP)
    fg = GATE_CAP * jnp.tanh(fg / GATE_CAP)
    ii = ig.reshape(bsz, nc, M_CHUNK, M_HEADS).transpose(0, 3, 1, 2)
    logf = jax.nn.log_sigmoid(fg).reshape(bsz, nc, M_CHUNK, M_HEADS).transpose(0, 3, 1, 2)

    bcum = jnp.cumsum(logf, axis=-1)
    b_last = bcum[..., -1]

    a = b_last[..., None] - bcum + ii
    m_loc = jnp.max(a, axis=-1)
    wgt = jnp.exp(a - m_loc[..., None])
    c_loc = jnp.einsum('bhcl,bhcld,bhcle->bhcde', wgt, k, v)
    n_loc = jnp.einsum('bhcl,bhcld->bhcd', wgt, k)

    def step(carry, inp):
        c_st, n_st, m_st = carry
        cl, nl, ml, bl = inp
        m_new = jnp.maximum(bl + m_st, ml)
        s_old = jnp.exp(bl + m_st - m_new)
        s_new = jnp.exp(ml - m_new)
        c_new = s_old[..., None, None] * c_st + s_new[..., None, None] * cl
        n_new = s_old[..., None] * n_st + s_new[..., None] * nl
        return (c_new, n_new, m_new), (c_st, n_st, m_st)

    init = (jnp.zeros((bsz, M_HEADS, M_DK, M_DV), jnp.float32),
            jnp.zeros((bsz, M_HEADS, M_DK), jnp.float32),
            jnp.zeros((bsz, M_HEADS), jnp.float32))
    xs = (jnp.moveaxis(c_loc, 2, 0), jnp.moveaxis(n_loc, 2, 0),
          jnp.moveaxis(m_loc, 2, 0), jnp.moveaxis(b_last, 2, 0))
    _, (c_prev, n_prev, m_prev) = lax.scan(step, init, xs)
    c_prev = jnp.moveaxis(c_prev, 0, 2)
    n_prev = jnp.moveaxis(n_prev, 0, 2)
    m_prev = jnp.moveaxis(m_prev, 0, 2)

    causal = jnp.tril(jnp.ones((M_CHUNK, M_CHUNK), dtype=bool))
    dmat = bcum[..., :, None] - bcum[..., None, :] + ii[..., None, :]
    dmat = jnp.where(causal, dmat, -jnp.inf)
    g_inter = bcum + m_prev[..., None]
    m_j = jnp.maximum(g_inter, jnp.max(dmat, axis=-1))
    decay = jnp.exp(dmat - m_j[..., None])
    scores = jnp.einsum('bhcid,bhcjd->bhcij', q, k) * decay
    inter = jnp.exp(g_inter - m_j)
    num = (inter[..., None] * jnp.einsum('bhcid,bhcde->bhcie', q, c_prev)
           + jnp.einsum('bhcij,bhcje->bhcie', scores, v))
    den = inter * jnp.einsum('bhcid,bhcd->bhci', q, n_prev) + jnp.sum(scores, axis=-1)
    hh = num / jnp.maximum(jnp.abs(den), jnp.exp(-m_j))[..., None]

    hh = hh * lax.rsqrt(jnp.mean(hh * hh, axis=-1, keepdims=True) + EPS)
    hh = hh.transpose(0, 2, 3, 1, 4).reshape(bsz, s, M_HEADS * M_DV)
    hh = hh * hnorm_g.astype(jnp.float32)
    out = jax.nn.sigmoid(o.astype(jnp.float32)) * hh
    return out.astype(h.dtype) @ w_out


def sgu_mixer(h, w_in, b_in, ln_g, ln_b, ws, bs, w_out):
    bsz, s, _ = h.shape
    nc = s // G_CHUNK
    z = jax.nn.gelu(h @ w_in + b_in, approximate=False)
    u, v = jnp.split(z, 2, axis=-1)
    v = layernorm(v, ln_g, ln_b)
    vr = v.reshape(bsz, nc, G_CHUNK, G_GROUPS, G_GROUP)
    wm = ws * jnp.tril(jnp.ones((G_CHUNK, G_CHUNK), ws.dtype))
    sv = jnp.einsum('gts,bnsgc->bntgc', wm, vr) + bs.T[:, :, None]
    y = u * sv.reshape(bsz, s, G_HALF)
    return y @ w_out


def setup_inputs(seed: int = 0) -> dict:
    key = jax.random.key(seed)
    ks = jax.random.split(key, 20)
    f32 = jnp.float32
    nrm = lambda k, shape, scale: jax.random.normal(k, shape, f32) * scale
    return {
        "x": nrm(ks[0], (BATCH, SEQ, D_MODEL), 1.0),
        "c": nrm(ks[1], (BATCH, D_MODEL), 1.0),
        "norm_g": 1.0 + nrm(ks[2], (DEPTH, 4, D_MODEL), 0.1),
        "ada_w": nrm(ks[3], (DEPTH, D_MODEL, 6 * D_MODEL), 0.5 * D_MODEL ** -0.5),
        "ada_b": nrm(ks[4], (DEPTH, 6 * D_MODEL), 0.02),
        "ffn_w1": nrm(ks[5], (DEPTH, D_MODEL, FFN_HIDDEN), D_MODEL ** -0.5),
        "ffn_w2": nrm(ks[6], (DEPTH, FFN_HIDDEN, D_MODEL), FFN_HIDDEN ** -0.5),
        "a_w_in": nrm(ks[7], (N_A, D_MODEL, A_IN), D_MODEL ** -0.5),
        "a_b_if": jnp.concatenate([nrm(ks[8], (N_A, M_HEADS), 0.1),
                                   3.0 + nrm(ks[9], (N_A, M_HEADS), 0.5)], axis=-1),
        "a_hnorm_g": 1.0 + nrm(ks[10], (N_A, M_HEADS * M_DV), 0.1),
        "a_w_out": nrm(ks[11], (N_A, M_HEADS * M_DV, D_MODEL), (M_HEADS * M_DV) ** -0.5),
        "b_w_in": nrm(ks[12], (N_B, D_MODEL, G_FFN), D_MODEL ** -0.5),
        "b_b_in": nrm(ks[13], (N_B, G_FFN), 0.02),
        "b_ln_g": 1.0 + nrm(ks[14], (N_B, G_HALF), 0.1),
        "b_ln_b": nrm(ks[15], (N_B, G_HALF), 0.02),
        "b_ws": nrm(ks[16], (N_B, G_GROUPS, G_CHUNK, G_CHUNK), G_CHUNK ** -0.5),
        "b_bs": 1.0 + nrm(ks[17], (N_B, G_GROUPS, G_CHUNK), 0.1),
        "b_w_out": nrm(ks[18], (N_B, G_HALF, D_MODEL), G_HALF ** -0.5),
    }


def reference(x, c, norm_g, ada_w, ada_b, ffn_w1, ffn_w2,
              a_w_in, a_b_if, a_hnorm_g, a_w_out,
              b_w_in, b_b_in, b_ln_g, b_ln_b, b_ws, b_bs, b_w_out):
    c_act = jax.nn.silu(c)
    for i in range(DEPTH):
        mod = c_act @ ada_w[i] + ada_b[i]
        sh1, sc1, g1, sh2, sc2, g2 = jnp.split(mod[:, None, :], 6, axis=-1)
        h = rmsnorm(x, norm_g[i, 0]) * (1.0 + sc1) + sh1
        j = i // N_MIXERS
        if i % N_MIXERS == 0:
            y = mlstm_mixer(h, a_w_in[j], a_b_if[j], a_hnorm_g[j], a_w_out[j])
        else:
            y = sgu_mixer(h, b_w_in[j], b_b_in[j], b_ln_g[j], b_ln_b[j], b_ws[j], b_bs[j], b_w_out[j])
        x = x + g1 * rmsnorm(y, norm_g[i, 1])
        h = rmsnorm(x, norm_g[i, 2]) * (1.0 + sc2) + sh2
        y = jnp.square(jax.nn.relu(h @ ffn_w1[i])) @ ffn_w2[i]
        x = x + g2 * rmsnorm(y, norm_g[i, 3])
    return x
```

```python
import numpy as np
import concourse.bass as bass
import concourse.mybir as mybir
from concourse.bass_utils import run_bass_kernel_spmd
from contextlib import ExitStack

F32 = mybir.dt.float32
BF16 = mybir.dt.bfloat16
AF = mybir.ActivationFunctionType
ALU = mybir.AluOpType
AX = mybir.AxisListType

D = 1024
KD = 8
NB = 2
DEPTH = 4
EPS = 1e-6
A_IN = 3088
ALL_SUBS = [(i, w) for i in range(DEPTH) for w in ("mix", "ffn")]


class Res:
    __slots__ = ("name", "lw", "rd")

    def __init__(self, name):
        self.name = name
        self.lw = None
        self.rd = []


class DSem:
    __slots__ = ("sem", "count")

    def __init__(self, sem):
        self.sem = sem
        self.count = 0


class Op:
    __slots__ = ("eng", "fn", "deps", "dma", "sig", "val", "signaled", "cnt")


class Prog:
    ENGS = ("pe", "act", "dve", "pool", "sp")

    def __init__(self):
        self.ops = []

    def op(self, eng, fn, reads=(), writes=(), dma=0, sig=None):
        i = len(self.ops)
        deps = set()
        for r in reads:
            if r.lw is not None:
                deps.add(r.lw)
        for w in writes:
            if w.lw is not None:
                deps.add(w.lw)
            deps.update(w.rd)
        deps.discard(i)
        o = Op()
        o.eng, o.fn, o.deps, o.dma, o.sig = eng, fn, deps, dma, sig
        o.signaled = False
        o.cnt = 0
        o.val = 0
        if dma:
            sig.count += 16 * dma
            o.val = sig.count
        for r in reads:
            r.rd.append(i)
        for w in writes:
            w.lw = i
            w.rd = []
        self.ops.append(o)
        return i

    def barrier(self, fns, res):
        n0 = getattr(self, "_last_bar", 0)
        dmas = [k for k in range(n0, len(self.ops)) if self.ops[k].dma]
        for e in ("pe", "act", "dve", "pool", "sp"):
            k = self.op(e, fns[e], writes=[res[e]])
            if e == "sp":
                self.ops[k].deps.update(dmas)
        for e in ("pe", "act", "dve", "pool", "sp"):
            self.op(e, fns[e], reads=[res[x] for x in res], writes=[res[e]])
        self._last_bar = len(self.ops)

    def emit(self, nc, block, sems):
        ops = self.ops
        for o in ops:
            for d in o.deps:
                p = ops[d]
                if p.dma:
                    continue
                if p.eng == o.eng and o.eng == "pe" and not o.dma:
                    continue
                p.signaled = True
        cnts = {e: 0 for e in self.ENGS}
        for o in ops:
            if o.signaled and not o.dma:
                cnts[o.eng] += 1
                o.cnt = cnts[o.eng]
        per = {e: [] for e in self.ENGS}
        for o in ops:
            per[o.eng].append(o)

        def run(engname):
            def body(e):
                waited = {}
                for o in per[engname]:
                    need = {}
                    for d in o.deps:
                        p = ops[d]
                        if p.dma:
                            key, val, s = id(p.sig), p.val, p.sig.sem
                        else:
                            if p.eng == engname and engname == "pe" and not o.dma:
                                continue
                            key, val, s = p.eng, p.cnt, sems[p.eng]
                        if need.get(key, (0, None))[0] < val:
                            need[key] = (val, s)
                    for key, (val, s) in need.items():
                        if waited.get(key, 0) < val:
                            e.wait_ge(s, val)
                            waited[key] = val
                    r = o.fn(e)
                    if o.dma:
                        assert len(r) == o.dma
                        for ins in r:
                            ins.then_inc(o.sig.sem, 16)
                    elif o.signaled:
                        r.then_inc(sems[engname], 1)
            return body

        block.tensor(run("pe"))
        block.scalar(run("act"))
        block.vector(run("dve"))
        block.gpsimd(run("pool"))
        block.sync(run("sp"))


def build_program(S, subs, debug_mod=False):
    NC = S // 128
    TOK = NB * S
    nc = bass.Bass("TRN2", target_bir_lowering=False)
    P = Prog()

    def din(name, shape):
        return nc.dram_tensor(name, list(shape), F32, kind="ExternalInput").ap()

    x_d = din("x", [TOK, D])
    cT_d = din("cT", [128, KD, NB])
    ngT_d = din("ngT", [128, DEPTH, 4, KD])
    ng_d = din("norm_g", [DEPTH, 4, D])
    adaw_d = din("ada_w", [DEPTH, D, 6 * D])
    adab_d = din("ada_b", [DEPTH, 6 * D])
    w1_d = din("ffn_w1", [DEPTH, D, 4 * D])
    w2_d = din("ffn_w2", [DEPTH, 4 * D, D])
    awin_d = din("a_w_in", [2, D, A_IN])
    abif_d = din("a_b_if", [2, 16])
    ahgT_d = din("a_hgT", [2, 128, 8])
    awout_d = din("a_w_out", [2, D, D])
    bwin_d = din("b_w_in", [2, D, 4 * D])
    bbinT_d = din("b_binT", [2, 128, 16])
    bbin_d = din("b_b_in", [2, 4 * D])
    blng_d = din("b_ln_g", [2, 2048])
    blnb_d = din("b_ln_b", [2, 2048])
    bwsT_d = din("b_wsT", [2, 128, 16, 128])
    bbs_d = din("b_bs", [2, 1, 2048])
    bwout_d = din("b_w_out", [2, 2048, D])
    ident_d = din("c_ident", [128, 128])
    triu_d = din("c_triu", [128, 128])
    mneg_d = din("c_mneg", [128, 128])
    out_d = nc.dram_tensor("out", [TOK, D], F32, kind="ExternalOutput").ap()
    xs_d = nc.dram_tensor("xs", [TOK, D], F32, kind="Internal").ap()
    mod_d = nc.dram_tensor("mod_d", [DEPTH, NB, 6 * D], F32, kind="Internal").ap()

    es = ExitStack()
    with es:
        def sb(name, shape, dt=F32):
            return es.enter_context(nc.sbuf_tensor(name, list(shape), dt))

        def pst(name, shape, dt=F32):
            return es.enter_context(nc.psum_tensor(name, list(shape), dt))

        def sem(name):
            return es.enter_context(nc.semaphore(name))

        sems = {e: sem("s_" + e) for e in ("pe", "act", "dve", "pool", "sp")}

        def dsem(name):
            return DSem(sem("d_" + name))

        wbuf = sb("wbuf", [128, 65536], BF16)
        NXS = 2
        xt = [sb(f"xt{i}", [128, D]) for i in range(NXS)]
        xt_r = [Res(f"xt{i}") for i in range(NXS)]
        xt_s = [dsem(f"xt{i}") for i in range(NXS)]
        xn = sb("xn", [128, D], BF16)
        xn_r = Res("xn")
        hT = [sb(f"hT{i}", [128, KD, 128], BF16) for i in range(2)]
        hT_r = [Res(f"hT{i}") for i in range(2)]
        big = sb("big", [128, 4096], BF16)
        big_r = Res("big")
        tt = sb("tt", [128, D])
        tt_r = Res("tt")
        tj, tj_r = tt, tt_r
        rbuf = [sb(f"rb{i}", [128, 512]) for i in range(2)]
        rbuf_r = [Res(f"rb{i}") for i in range(2)]
        gbc = sb("gbc", [128, D])
        gbc_r = Res("gbc")
        gbc_s = dsem("gbc")
        ngbc, ngbc_r = tt, tt_r
        ngbc_s = dsem("ngbc")
        st = sb("st", [128, 64])
        st_r = Res("st")
        stt_ = sb("stt_", [128, 64])
        modT = sb("modT", [128, DEPTH, NB, 6, KD])
        modT_r = Res("modT")
        modT_s = dsem("modT")
        ASb = sb("AS", [128, DEPTH, 4, KD, NB])
        AS_r = Res("AS")
        ngT = sb("ngTs", [128, DEPTH, 4, KD])
        cons_r = Res("cons")
        cons_s = dsem("cons")
        cact = sb("cact", [128, KD, NB])
        identb = sb("identb", [128, 128], BF16)
        triu = sb("triu", [128, 128])
        mneg = sb("mneg", [128, 128])
        ones_f = sb("ones_f", [128, 128])
        ones_b = sb("ones_b", [128, 128], BF16)
        epst = sb("epst", [128, 1])
        one1 = sb("one1", [128, 1])
        adab = wbuf[0:NB, 32768:32768 + 12288].bitcast(F32)
        modsb = wbuf[0:NB, 45056:45056 + 12288].bitcast(F32)
        modsb_r = Res("modsb")
        modsb_s = dsem("modsb")
        mixbuf = sb("mixbuf", [128, 18432], BF16)

        def carver():
            cur = [0]

            def carve(name, shape, dt=F32):
                n = int(np.prod(shape[1:]))
                nb = n * (4 if dt == F32 else 2)
                a = cur[0]
                cur[0] += ((nb + 63) // 64 * 64) // 2
                assert cur[0] <= 18432, name
                v = mixbuf[:, a:a + nb // 2]
                if dt == F32:
                    v = v.bitcast(F32)
                if len(shape) == 3:
                    v = v.rearrange("p (a b) -> p a b", a=shape[1])
                return v
            return carve
        sbs = carver()
        sbm = carver()
        v32 = sbs("v32", [128, 2048])
        v32_r = Res("v32")
        vnb = sbs("vnb", [128, 2048], BF16)
        vnb_r = Res("vnb")
        uT = sbs("uT", [128, 16, 128])
        sgT_m = sbm("sgT", [128, 8, 128])
        uT_r = Res("uT")
        lng = sbs("lng", [128, 2048])
        lnb = sbs("lnb", [128, 2048])
        lnp_r = Res("lnp")
        lnp_s = dsem("lnp")
        smallw = sb("smallw", [128, 64])
        smallw_r = Res("smallw")
        smallw_s = dsem("smallw")
        rowb = wbuf[0:1, 51200:57344]
        rowf = wbuf[0:1, 57344:65536].bitcast(F32)
        row_s = dsem("row")
        qT = sbm("qT", [128, 4, 128], BF16)
        kT = sbm("kT", [128, 4, 128], BF16)
        qsT = sbm("qsT", [128, 4, 128], BF16)
        qk_r = Res("qk")
        qs_r = [Res(f"qs{h}") for h in range(8)]
        kw = sbm("kw", [128, 8, 64], BF16)
        kw_r = Res("kw")
        vaug = sbm("vaug", [128, 8, 129], BF16)
        vaug_r = Res("vaug")
        gts = sbm("gts", [128, 128])
        gts_r = Res("gts")
        lrep = sbm("lrep", [128, 8, 128])
        lrep_r = Res("lrep")
        Cst = sbm("Cst", [128, 4, 129])
        Cst_r = Res("Cst")
        Cbf = sbm("Cbf", [128, 4, 129], BF16)
        nrep = sbm("nrep", [128, 4, 128], BF16)
        Cbf_r = Res("Cbf")
        hd = []
        for i in range(8):
            _a, _d, _e = sbm(f"argm{i}", [128, 128]), sbm(f"dec{i}", [128, 128]), sbm(f"ebh{i}", [128, 128])
            hd.append(dict(argm=_a, dec=_d, eb=_e, sc=sbm(f"sc{i}", [128, 128], BF16), dn=_a, hh=_d,
                           sq=sbm(f"sq{i}", [128, 128], BF16), rs=_e, r=Res(f"hd{i}")))
        wtmp = v32[:].rearrange("p (g t) -> p g t", g=16)
        wtmp_r = v32_r
        wtmp_s = dsem("wtmp")

        psT = pst("psT", [128, 1024], BF16)
        psT_r = Res("psT")
        psY = pst("psY", [128, 1024])
        psY_r = Res("psY")
        NG = 5
        gp = [pst(f"gp{i}", [128, 512]) for i in range(NG)]
        gp_r = [Res(f"gp{i}") for i in range(NG)]
        gctr = [0]

        def bank():
            i = gctr[0] % NG
            gctr[0] += 1
            return gp[i], gp_r[i]

        W_r = [Res("W0"), Res("W1")]
        W_s = [dsem("W0"), dsem("W1")]

        bar_t = sb("bar_t", [128, 8])
        bar_res = {e: Res("bar_" + e) for e in ("pe", "act", "dve", "pool", "sp")}
        bar_fns = {
            "pe": lambda e: e.matmul(psY[0:1, 0:2], lhsT=identb[:, 0:1], rhs=identb[:, 0:2], start=True, stop=True),
            "act": lambda e: e.activation(out=bar_t[:, 0:1], in_=epst[:, 0:1], func=AF.Identity),
            "dve": lambda e: e.memset(bar_t[:, 2:3], 0.0),
            "pool": lambda e: e.memset(bar_t[:, 4:5], 0.0),
            "sp": lambda e: e.nop(),
        }
        bar_res["pe"] = psY_r
        def load_consts(e):
            return [
                e.dma_start(out=cact[:], in_=cT_d),
                e.dma_start(out=ngT[:], in_=ngT_d),
                e.dma_start(out=triu[:], in_=triu_d),
                e.dma_start(out=mneg[:], in_=mneg_d),
            ]
        P.op("sp", load_consts, writes=[cons_r], dma=4, sig=cons_s)
        P.op("pool", lambda e: [e.dma_start(out=identb[:], in_=ident_d)], writes=[cons_r], dma=1, sig=cons_s)
        P.op("dve", lambda e: e.memset(ones_f[:], 1.0), writes=[cons_r])
        P.op("dve", lambda e: e.memset(ones_b[:], 1.0), writes=[cons_r])
        P.op("dve", lambda e: e.memset(epst[:], EPS), writes=[cons_r])
        P.op("dve", lambda e: e.memset(one1[:], 1.0), writes=[cons_r])
        P.op("act", lambda e: e.activation(out=cact[:], in_=cact[:], func=AF.Silu), reads=[cons_r], writes=[cons_r])

        layers_used = sorted(set(i for i, _ in subs))
        adaslot = [wbuf[:, s * 8192:(s + 1) * 8192].bitcast(F32).rearrange("p (k n) -> p k n", k=KD) for s in range(3)]
        ada_r = [Res(f"ada{s}") for s in range(3)]
        ada_s = [dsem(f"ada{s}") for s in range(3)]
        blk = 0
        for i in layers_used:
            def ld_adab(e, i=i):
                return [e.dma_start(out=adab[:], in_=adab_d[i:i + 1, :].partition_broadcast(NB))]
            P.op("sp", ld_adab, writes=[modsb_r], dma=1, sig=modsb_s)
            for j in range(12):
                s = blk % 3
                blk += 1

                def ld(e, i=i, j=j, s=s):
                    return [e.dma_start(out=adaslot[s][:, k, :], in_=adaw_d[i, k * 128:(k + 1) * 128, j * 512:(j + 1) * 512])
                            for k in range(KD)]
                P.op("sp", ld, writes=[ada_r[s]], dma=KD, sig=ada_s[s])
                pb, pb_r = bank()

                def mm(e, s=s, pb=pb):
                    for k in range(KD):
                        r = e.matmul(pb[0:NB, :], lhsT=cact[:, k, :], rhs=adaslot[s][:, k, :], start=(k == 0), stop=(k == KD - 1))
                    return r
                P.op("pe", mm, reads=[ada_r[s], cons_r], writes=[pb_r])
                P.op("dve", lambda e, j=j, pb=pb: e.tensor_tensor(out=modsb[:, j * 512:(j + 1) * 512], in0=pb[0:NB, :],
                                                                  in1=adab[:, j * 512:(j + 1) * 512], op=ALU.add),
                     reads=[pb_r], writes=[modsb_r])
            P.op("sp", lambda e, i=i: [e.dma_start(out=mod_d[i], in_=modsb[:])], reads=[modsb_r], writes=[modT_r],
                 dma=1, sig=modsb_s)
            for b in range(NB):
                def ldT(e, i=i, b=b):
                    return [e.dma_start(out=modT[:, i, b, v, :],
                                        in_=mod_d[i, b, v * D:(v + 1) * D].rearrange("(k p) -> p k", p=128),
                                        allow_slow_non_contiguous=True) for v in range(6)]
                P.op("sp", ldT, reads=[modT_r], writes=[modT_r], dma=6, sig=modT_s)
            for b in range(NB):
                for (w, vsc, vsh, j) in ((0, 1, 0, 0), (2, 4, 3, 2)):
                    P.op("dve", lambda e, i=i, b=b, w=w, vsc=vsc, j=j: e.scalar_tensor_tensor(
                        out=ASb[:, i, w, :, b], in0=modT[:, i, b, vsc, :], scalar=1.0, in1=ngT[:, i, j, :],
                        op0=ALU.add, op1=ALU.mult), reads=[modT_r, cons_r], writes=[AS_r])
                    P.op("dve", lambda e, i=i, b=b, w=w, vsh=vsh: e.tensor_copy(out=ASb[:, i, w + 1, :, b], in_=modT[:, i, b, vsh, :]),
                         reads=[modT_r], writes=[AS_r])
        for s in range(3):
            W_r[0].rd += ada_r[s].rd
            if ada_r[s].lw is not None:
                W_r[0].rd.append(ada_r[s].lw)
        W_r[1].rd += modsb_r.rd
        if modsb_r.lw is not None:
            W_r[1].rd.append(modsb_r.lw)

        xctr = [0]
        hctr = [0]

        def src_dst(si):
            src = x_d if si == 0 else xs_d
            dst = out_d if si == len(subs) - 1 else xs_d
            return src, dst
        dram_r = [Res(f"rows{t}") for t in range(NB * NC)]

        stf_r = [Res("stf0"), Res("stf1")]
        stb_r = Res("stb")

        def front_a(si, i, which, b, t):
            src, _ = src_dst(si)
            xs_ = xctr[0] % NXS
            xctr[0] += 1
            st_r = stf_r[xs_]
            st = stt_[:, 32 * xs_:32 * xs_ + 32]
            P.op("sp", lambda e: [e.dma_start(out=xt[xs_][:], in_=src[t * 128:(t + 1) * 128, :])],
                 reads=[dram_r[t]], writes=[xt_r[xs_]], dma=1, sig=xt_s[xs_])
            P.op("act", lambda e: e.activation(out=xn[:], in_=xt[xs_][:], func=AF.Square, accum_out=st[:, 0:1]),
                 reads=[xt_r[xs_]], writes=[xn_r, st_r])
            P.op("act", lambda e: e.activation(out=st[:, 1:2], in_=st[:, 0:1], func=AF.Sqrt, scale=1.0 / D, bias=epst[:, 0:1]),
                 reads=[st_r, cons_r], writes=[st_r])
            P.op("dve", lambda e: e.reciprocal(out=st[:, 2:3], in_=st[:, 1:2]), reads=[st_r], writes=[st_r])
            P.op("dve", lambda e: e.tensor_scalar(out=xn[:], in0=xt[xs_][:], scalar1=st[:, 2:3], scalar2=None, op0=ALU.mult),
                 reads=[xt_r[xs_], st_r], writes=[xn_r])
            return xs_

        def front_b(i, which, b):
            hs_ = hctr[0] % 2
            hctr[0] += 1
            w = 0 if which == "mix" else 2

            def tr(e):
                for k in range(KD):
                    r = e.transpose(out=psT[:, k * 128:(k + 1) * 128], in_=xn[:, k * 128:(k + 1) * 128], identity=identb[:])
                return r
            P.op("pe", tr, reads=[xn_r, cons_r], writes=[psT_r])
            for k in range(KD):
                if k % 2 == 0:
                    P.op("act", lambda e, k=k: e.activation(out=hT[hs_][:, k, :], in_=psT[:, k * 128:(k + 1) * 128], func=AF.Identity,
                                                            scale=ASb[:, i, w, k, b:b + 1], bias=ASb[:, i, w + 1, k, b:b + 1]),
                         reads=[psT_r, AS_r], writes=[hT_r[hs_]])
                else:
                    P.op("dve", lambda e, k=k: e.tensor_scalar(out=hT[hs_][:, k, :], in0=psT[:, k * 128:(k + 1) * 128],
                                                               scalar1=ASb[:, i, w, k, b:b + 1], scalar2=ASb[:, i, w + 1, k, b:b + 1],
                                                               op0=ALU.mult, op1=ALU.add),
                         reads=[psT_r, AS_r], writes=[hT_r[hs_]])
            return hs_

        def drive(si, i, which, middle, seq_init=None):
            seq = [(b, c) for b in range(NB) for c in range(NC)]
            slots = {}
            slots[0] = [front_a(si, i, which, 0, 0), None]
            slots[0][1] = front_b(i, which, 0)
            for idx, (b, c) in enumerate(seq):
                t = b * NC + c
                if c == 0:
                    load_gate(i, which, b)
                    if seq_init is not None:
                        seq_init(b)
                nxt = seq[idx + 1] if idx + 1 < len(seq) else None
                if nxt is not None:
                    slots[t + 1] = [front_a(si, i, which, nxt[0], t + 1), None]
                middle(b, c, t, slots[t][0], slots[t][1])
                if nxt is not None:
                    slots[t + 1][1] = front_b(i, which, nxt[0])
                back(si, t, slots[t][0])

        def back(si, t, xs_):
            _, dst = src_dst(si)
            P.op("act", lambda e: e.activation(out=tj[:], in_=psY[:], func=AF.Square, accum_out=st[:, 4:5]),
                 reads=[psY_r], writes=[tj_r, stb_r])
            P.op("act", lambda e: e.activation(out=st[:, 5:6], in_=st[:, 4:5], func=AF.Sqrt, scale=1.0 / D, bias=epst[:, 0:1]),
                 reads=[stb_r, cons_r], writes=[stb_r])
            P.op("dve", lambda e: e.reciprocal(out=st[:, 6:7], in_=st[:, 5:6]), reads=[stb_r], writes=[stb_r])
            P.op("dve", lambda e: e.scalar_tensor_tensor(out=tt[:], in0=psY[:], scalar=st[:, 6:7], in1=gbc[:], op0=ALU.mult, op1=ALU.mult),
                 reads=[psY_r, stb_r, gbc_r], writes=[tt_r])
            P.op("pool", lambda e: e.tensor_tensor(out=xt[xs_][:], in0=xt[xs_][:], in1=tt[:], op=ALU.add),
                 reads=[tt_r, xt_r[xs_]], writes=[xt_r[xs_]])
            P.op("sp", lambda e: [e.dma_start(out=dst[t * 128:(t + 1) * 128, :], in_=xt[xs_][:])],
                 reads=[xt_r[xs_]], writes=[dram_r[t]], dma=1, sig=xt_s[xs_])

        def load_gate(i, which, b):
            v = 2 if which == "mix" else 5
            j = 1 if which == "mix" else 3
            P.op("sp", lambda e: [e.dma_start(out=gbc[:], in_=mod_d[i, b:b + 1, v * D:(v + 1) * D].partition_broadcast(128))],
                 reads=[modT_r], writes=[gbc_r], dma=1, sig=gbc_s)
            P.op("sp", lambda e: [e.dma_start(out=ngbc[:], in_=ng_d[i, j:j + 1, :].partition_broadcast(128))],
                 writes=[ngbc_r], dma=1, sig=ngbc_s)
            P.op("dve", lambda e: e.tensor_tensor(out=gbc[:], in0=gbc[:], in1=ngbc[:], op=ALU.mult),
                 reads=[gbc_r, ngbc_r], writes=[gbc_r])

        def wview(off, k, n):
            return wbuf[:, off:off + k * n].rearrange("p (k n) -> p k n", k=k)

        def ffn(si, i):
            W1 = wview(0, KD, 4096)
            W2 = wview(32768, 32, D)
            P.op("pool", lambda e: [e.dma_start(out=W1[:, k, :], in_=w1_d[i, k * 128:(k + 1) * 128, :]) for k in range(KD)],
                 writes=[W_r[0]], dma=KD, sig=W_s[0])
            P.op("pool", lambda e: [e.dma_start(out=W2[:, 4 * k:4 * k + 4, :],
                                                in_=w2_d[i, k * 512:(k + 1) * 512, :].rearrange("(c p) n -> p c n", p=128))
                                    for k in range(8)],
                 writes=[W_r[1]], dma=8, sig=W_s[1])
            hidT = big[:].rearrange("p (c n) -> p c n", c=32)
            def middle(b, c, t, xs_, hs_):
                for q in range(8):
                    pb, pb_r = bank()

                    def mm(e, q=q, pb=pb):
                        for cc in range(4):
                            hc = q * 4 + cc
                            for k in range(KD):
                                r = e.matmul(pb[:, cc * 128:(cc + 1) * 128], lhsT=W1[:, k, hc * 128:(hc + 1) * 128],
                                             rhs=hT[hs_][:, k, :], start=(k == 0), stop=(k == KD - 1))
                        return r
                    P.op("pe", mm, reads=[W_r[0], hT_r[hs_]], writes=[pb_r])
                    rs_ = q % 2
                    P.op("act", lambda e, pb=pb, rs_=rs_: e.activation(out=rbuf[rs_][:], in_=pb[:], func=AF.Relu),
                         reads=[pb_r], writes=[rbuf_r[rs_]])
                    P.op("pool", lambda e, q=q, rs_=rs_: e.tensor_tensor(
                        out=big[:, q * 512:(q + 1) * 512], in0=rbuf[rs_][:], in1=rbuf[rs_][:], op=ALU.mult),
                        reads=[rbuf_r[rs_]], writes=[big_r])

                def mm2(e):
                    for n in range(2):
                        for hc in range(32):
                            r = e.matmul(psY[:, n * 512:(n + 1) * 512], lhsT=hidT[:, hc, :], rhs=W2[:, hc, n * 512:(n + 1) * 512],
                                         start=(hc == 0), stop=(hc == 31))
                    return r
                P.op("pe", mm2, reads=[W_r[1], big_r], writes=[psY_r])
            drive(si, i, "ffn", middle)

        def sgu(si, i):
            j = i // 2
            Win = wview(0, KD, 4096)
            Wout = wview(32768, 16, D)
            wmT = wview(32768 + 16384, 16, 128)
            P.op("pool", lambda e: [e.dma_start(out=Win[:, k, :], in_=bwin_d[j, k * 128:(k + 1) * 128, :]) for k in range(KD)],
                 writes=[W_r[0]], dma=KD, sig=W_s[0])
            P.op("pool", lambda e: [e.dma_start(out=Wout[:, 4 * k:4 * k + 4, :],
                                                in_=bwout_d[j, k * 512:(k + 1) * 512, :].rearrange("(c p) n -> p c n", p=128))
                                    for k in range(4)],
                 writes=[W_r[1]], dma=4, sig=W_s[1])
            P.op("sp", lambda e: [e.dma_start(out=wtmp[:], in_=bwsT_d[j])], writes=[wtmp_r], dma=1, sig=wtmp_s)
            for g in range(16):
                P.op("pool", lambda e, g=g: e.tensor_tensor(out=wmT[:, g, :], in0=wtmp[:, g, :], in1=(triu[:]), op=ALU.mult),
                     reads=[wtmp_r, cons_r], writes=[W_r[1]])
            P.op("sp", lambda e: [e.dma_start(out=lng[:], in_=blng_d[j:j + 1, :].partition_broadcast(128)),
                                  e.dma_start(out=lnb[:], in_=blnb_d[j:j + 1, :].partition_broadcast(128))],
                 writes=[lnp_r], dma=2, sig=lnp_s)
            P.op("sp", lambda e: [e.dma_start(out=smallw[:, 0:16], in_=bbinT_d[j])], writes=[smallw_r], dma=1, sig=smallw_s)
            P.op("pool", lambda e: [e.dma_start(out=rowb[0:1, 0:2048], in_=bbin_d[j:j + 1, 2048:4096]),
                                    e.dma_start(out=rowb[0:1, 2048:4096], in_=bbs_d[j])],
                 writes=[W_r[1]], dma=2, sig=row_s)
            P.op("sp", lambda e: [e.dma_start(out=rowf[0:1, 0:2048], in_=bbs_d[j])], writes=[W_r[1]], dma=1, sig=row_s)
            P.op("dve", lambda e: e.tensor_copy(out=rowf[0:1, 2048:4096], in_=rowb[0:1, 2048:4096]), reads=[W_r[1]], writes=[W_r[1]])
            P.op("dve", lambda e: e.tensor_tensor(out=rowb[0:1, 4096:6144], in0=rowf[0:1, 0:2048], in1=rowf[0:1, 2048:4096], op=ALU.subtract),
                 reads=[W_r[1]], writes=[W_r[1]])
            bu_row = wbuf[0:1, 57344:57344 + 2048]
            P.op("pool", lambda e: [e.dma_start(out=bu_row, in_=bbin_d[j:j + 1, 0:2048])], reads=[W_r[1]], writes=[W_r[1]],
                 dma=1, sig=row_s)
            yT = big[:, 0:2048].rearrange("p (c n) -> p c n", c=16)
            def middle(b, c, t, xs_, hs_):
                for n in range(4):
                    pb, pb_r = bank()

                    def mmv(e, n=n, pb=pb):
                        for k in range(KD):
                            e.matmul(pb[:], lhsT=hT[hs_][:, k, :], rhs=Win[:, k, 2048 + n * 512:2048 + (n + 1) * 512],
                                     start=(k == 0), stop=False)
                        return e.matmul(pb[:], lhsT=ones_b[0:1, :], rhs=rowb[0:1, n * 512:(n + 1) * 512], start=False, stop=True)
                    P.op("pe", mmv, reads=[W_r[0], hT_r[hs_], W_r[1], cons_r], writes=[pb_r])
                    P.op("act", lambda e, n=n, pb=pb: e.activation(out=v32[:, n * 512:(n + 1) * 512], in_=pb[:], func=AF.Gelu,
                                                                   accum_out=st[:, 8 + n:9 + n]),
                         reads=[pb_r], writes=[v32_r, st_r])
                P.op("act", lambda e: e.activation(out=vnb[:], in_=v32[:], func=AF.Square, accum_out=st[:, 12:13]),
                     reads=[v32_r], writes=[vnb_r, st_r])
                P.op("dve", lambda e: e.reduce_sum(out=st[:, 13:14], in_=st[:, 8:12], axis=AX.X), reads=[st_r], writes=[st_r])
                P.op("dve", lambda e: e.tensor_scalar(out=st[:, 14:15], in0=st[:, 13:14], scalar1=1.0 / 2048, scalar2=None, op0=ALU.mult),
                     reads=[st_r], writes=[st_r])
                P.op("dve", lambda e: e.tensor_tensor(out=st[:, 15:16], in0=st[:, 14:15], in1=st[:, 14:15], op=ALU.mult),
                     reads=[st_r], writes=[st_r])
                P.op("dve", lambda e: e.scalar_tensor_tensor(out=st[:, 16:17], in0=st[:, 12:13], scalar=1.0 / 2048, in1=st[:, 15:16],
                                                             op0=ALU.mult, op1=ALU.subtract), reads=[st_r], writes=[st_r])
                P.op("act", lambda e: e.activation(out=st[:, 17:18], in_=st[:, 16:17], func=AF.Sqrt, bias=epst[:, 0:1]),
                     reads=[st_r, cons_r], writes=[st_r])
                P.op("dve", lambda e: e.reciprocal(out=st[:, 18:19], in_=st[:, 17:18]), reads=[st_r], writes=[st_r])
                P.op("dve", lambda e: e.scalar_tensor_tensor(out=st[:, 19:20], in0=st[:, 14:15], scalar=-1.0, in1=st[:, 18:19],
                                                             op0=ALU.mult, op1=ALU.mult), reads=[st_r], writes=[st_r])
                P.op("act", lambda e: e.activation(out=v32[:], in_=v32[:], func=AF.Identity, scale=st[:, 18:19], bias=st[:, 19:20]),
                     reads=[v32_r, st_r], writes=[v32_r])
                P.op("pool", lambda e: e.tensor_tensor(out=v32[:], in0=v32[:], in1=lng[:], op=ALU.mult),
                     reads=[v32_r, lnp_r], writes=[v32_r])
                P.op("dve", lambda e: e.tensor_tensor(out=vnb[:], in0=v32[:], in1=lnb[:], op=ALU.add),
                     reads=[v32_r, lnp_r], writes=[vnb_r])
                for q in range(4):
                    pb, pb_r = bank()

                    def mm(e, q=q, pb=pb):
                        for cc in range(4):
                            uc = q * 4 + cc
                            for k in range(KD):
                                e.matmul(pb[:, cc * 128:(cc + 1) * 128], lhsT=Win[:, k, uc * 128:(uc + 1) * 128],
                                         rhs=hT[hs_][:, k, :], start=(k == 0), stop=False)
                            r = e.matmul(pb[:, cc * 128:(cc + 1) * 128], lhsT=bu_row[0:1, uc * 128:(uc + 1) * 128],
                                         rhs=ones_b[0:1, 0:128], start=False, stop=True)
                        return r
                    P.op("pe", mm, reads=[W_r[0], W_r[1], hT_r[hs_], cons_r], writes=[pb_r])
                    P.op("act", lambda e, pb=pb, q=q: e.activation(
                        out=uT[:, q * 4:(q + 1) * 4, :].rearrange("p c n -> p (c n)"), in_=pb[:], func=AF.Gelu),
                        reads=[pb_r], writes=[uT_r])
                for q in range(4):
                    pb, pb_r = bank()

                    def mms(e, q=q, pb=pb):
                        for cc in range(4):
                            g = q * 4 + cc
                            o = pb[:, cc * 128:(cc + 1) * 128]
                            e.matmul(o, lhsT=vnb[:, g * 128:(g + 1) * 128], rhs=wmT[:, g, :], start=True, stop=False)
                            e.matmul(o, lhsT=ones_b[0:1, :], rhs=rowb[0:1, 2048 + g * 128:2048 + (g + 1) * 128], start=False, stop=False)
                            r = e.matmul(o, lhsT=ones_b[0:1, :], rhs=rowb[0:1, 4096 + g * 128:4096 + (g + 1) * 128], start=False, stop=True)
                        return r
                    P.op("pe", mms, reads=[vnb_r, W_r[1], W_r[1], cons_r], writes=[pb_r])
                    P.op("dve", lambda e, q=q, pb=pb: e.tensor_tensor(
                        out=big[:, q * 512:(q + 1) * 512], in0=uT[:, q * 4:(q + 1) * 4, :].rearrange("p c n -> p (c n)"),
                        in1=pb[:], op=ALU.mult), reads=[uT_r, pb_r], writes=[big_r])

                def mm2(e):
                    for n in range(2):
                        for cc in range(16):
                            r = e.matmul(psY[:, n * 512:(n + 1) * 512], lhsT=yT[:, cc, :], rhs=Wout[:, cc, n * 512:(n + 1) * 512],
                                         start=(cc == 0), stop=(cc == 15))
                    return r
                P.op("pe", mm2, reads=[W_r[1], big_r], writes=[psY_r])
            drive(si, i, "mix", middle)

        def mlstm(si, i):
            j = i // 2
            Win = wview(0, KD, A_IN)
            Wout = wview(32768, KD, D)
            P.op("pool", lambda e: [e.dma_start(out=Win[:, k, :], in_=awin_d[j, k * 128:(k + 1) * 128, :]) for k in range(KD)],
                 writes=[W_r[0]], dma=KD, sig=W_s[0])
            P.op("pool", lambda e: [e.dma_start(out=Wout[:, 4 * k:4 * k + 4, :],
                                                in_=awout_d[j, k * 512:(k + 1) * 512, :].rearrange("(c p) n -> p c n", p=128))
                                    for k in range(2)],
                 writes=[W_r[1]], dma=2, sig=W_s[1])
            P.op("sp", lambda e: [e.dma_start(out=smallw[:, 16:24], in_=ahgT_d[j]),
                                  e.dma_start(out=smallw[:, 32:48], in_=abif_d[j:j + 1, :].partition_broadcast(128))],
                 writes=[smallw_r], dma=2, sig=smallw_s)
            gT = big[:, 0:1024].rearrange("p (c n) -> p c n", c=8)
            sgT = sgT_m
            def middle(b, c, t, xs_, hs_):
                pbg, pbg_r = bank()

                def mmg(e, pbg=pbg):
                    for k in range(KD):
                        r = e.matmul(pbg[:, 0:16], lhsT=hT[hs_][:, k, :], rhs=Win[:, k, 3072:3088], start=(k == 0), stop=(k == KD - 1))
                    return r
                P.op("pe", mmg, reads=[W_r[0], hT_r[hs_]], writes=[pbg_r])
                P.op("dve", lambda e, pbg=pbg: e.tensor_tensor(out=gts[:, 0:16], in0=pbg[:, 0:16], in1=smallw[:, 32:48], op=ALU.add),
                     reads=[pbg_r, smallw_r], writes=[gts_r])
                P.op("act", lambda e: e.activation(out=gts[:, 16:32], in_=gts[:, 0:16], func=AF.Tanh, scale=1.0 / 15.0),
                     reads=[gts_r], writes=[gts_r])
                P.op("dve", lambda e: e.tensor_scalar(out=gts[:, 32:40], in0=gts[:, 16:24], scalar1=15.0, scalar2=None, op0=ALU.mult),
                     reads=[gts_r], writes=[gts_r])
                P.op("act", lambda e: e.activation(out=gts[:, 40:48], in_=gts[:, 24:32], func=AF.Exp, scale=-15.0),
                     reads=[gts_r], writes=[gts_r])
                P.op("act", lambda e: e.activation(out=gts[:, 48:56], in_=gts[:, 40:48], func=AF.Ln, bias=one1[:, 0:1]),
                     reads=[gts_r, cons_r], writes=[gts_r])
                P.op("dve", lambda e: e.tensor_scalar(out=gts[:, 48:56], in0=gts[:, 48:56], scalar1=-1.0, scalar2=None, op0=ALU.mult),
                     reads=[gts_r], writes=[gts_r])
                for (col0, dstT, scl) in ((0, qT, 1.0), (512, kT, 0.125)):
                    pb, pb_r = bank()

                    def mm(e, col0=col0, pb=pb):
                        for cc in range(4):
                            for k in range(KD):
                                r = e.matmul(pb[:, cc * 128:(cc + 1) * 128], lhsT=Win[:, k, col0 + cc * 128:col0 + (cc + 1) * 128],
                                             rhs=hT[hs_][:, k, :], start=(k == 0), stop=(k == KD - 1))
                        return r
                    P.op("pe", mm, reads=[W_r[0], hT_r[hs_]], writes=[pb_r])
                    P.op("act", lambda e, pb=pb, dstT=dstT, scl=scl: e.activation(
                        out=dstT[:].rearrange("p c n -> p (c n)"), in_=pb[:], func=AF.Identity, scale=scl),
                        reads=[pb_r], writes=[qk_r])
                for q in range(2):
                    pb, pb_r = bank()

                    def mmo(e, q=q, pb=pb):
                        for cc in range(4):
                            oc = 2048 + (q * 4 + cc) * 128
                            for k in range(KD):
                                r = e.matmul(pb[:, cc * 128:(cc + 1) * 128], lhsT=Win[:, k, oc:oc + 128],
                                             rhs=hT[hs_][:, k, :], start=(k == 0), stop=(k == KD - 1))
                        return r
                    P.op("pe", mmo, reads=[W_r[0], hT_r[hs_]], writes=[pb_r])
                    P.op("act", lambda e, q=q, pb=pb: e.activation(
                        out=sgT[:, q * 4:(q + 1) * 4, :].rearrange("p c n -> p (c n)"), in_=pb[:], func=AF.Sigmoid),
                        reads=[pb_r], writes=[uT_r])
                pbk, pbk_r = bank()

                def mmk(e, pbk=pbk):
                    for k in range(KD):
                        r = e.matmul(pbk[:], lhsT=hT[hs_][:, k, :], rhs=Win[:, k, 512:1024], start=(k == 0), stop=(k == KD - 1))
                    return r
                P.op("pe", mmk, reads=[W_r[0], hT_r[hs_]], writes=[pbk_r])
                for n in range(2):
                    pb, pb_r = bank()

                    def mmv(e, n=n, pb=pb):
                        for k in range(KD):
                            r = e.matmul(pb[:], lhsT=hT[hs_][:, k, :], rhs=Win[:, k, 1024 + n * 512:1024 + (n + 1) * 512],
                                         start=(k == 0), stop=(k == KD - 1))
                        return r
                    P.op("pe", mmv, reads=[W_r[0], hT_r[hs_]], writes=[pb_r])
                    P.op("act", lambda e, n=n, pb=pb: e.activation(
                        out=vaug[:, n * 4:(n + 1) * 4, 0:128], in_=pb[:].rearrange("p (c n) -> p c n", c=4), func=AF.Identity),
                        reads=[pb_r], writes=[vaug_r])
                pbc, pbc_r = bank()

                def mmc(e, pbc=pbc):
                    e.matmul(pbc[:, 0:8], lhsT=triu[:], rhs=gts[:, 48:56], start=True, stop=True)
                    return e.matmul(pbc[:, 8:16], lhsT=ones_f[:], rhs=gts[:, 48:56], start=True, stop=True)
                P.op("pe", mmc, reads=[gts_r, cons_r], writes=[pbc_r])
                P.op("dve", lambda e, pbc=pbc: e.tensor_copy(out=gts[:, 56:72], in_=pbc[:, 0:16]), reads=[pbc_r], writes=[gts_r])
                P.op("dve", lambda e: e.tensor_tensor(out=gts[:, 80:88], in0=gts[:, 32:40], in1=gts[:, 56:64], op=ALU.subtract),
                     reads=[gts_r], writes=[gts_r])
                P.op("dve", lambda e: e.tensor_tensor(out=gts[:, 72:80], in0=gts[:, 80:88], in1=gts[:, 64:72], op=ALU.add),
                     reads=[gts_r], writes=[gts_r])
                P.op("act", lambda e: e.activation(out=gts[:, 72:80], in_=gts[:, 72:80], func=AF.Exp), reads=[gts_r], writes=[gts_r])
                P.op("act", lambda e: e.activation(out=gts[:, 88:96], in_=gts[:, 64:72], func=AF.Exp), reads=[gts_r], writes=[gts_r])
                for h in range(8):
                    P.op("dve", lambda e, pbk=pbk, h=h: e.tensor_scalar(
                        out=kw[:, h, :], in0=pbk[:, h * 64:(h + 1) * 64], scalar1=gts[:, 72 + h:73 + h], scalar2=0.125,
                        op0=ALU.mult, op1=ALU.mult), reads=[pbk_r, gts_r], writes=[kw_r])
                for h in range(8):
                    P.op("pool", lambda e, h=h: e.tensor_scalar(out=lrep[:, h, :], in0=ones_f[:], scalar1=gts[:, 48 + h:49 + h],
                                                                scalar2=None, op0=ALU.mult),
                         reads=[gts_r, cons_r], writes=[lrep_r])
                def head(h, H):
                    hh_, r0 = h // 2, (h % 2) * 64
                    while not hfree:
                        yield
                    ipB = hfree.pop(0)
                    pB, pB_r = gp[ipB], gp_r[ipB]
                    yield
                    P.op("pe", lambda e, h=h, pB=pB: e.matmul(pB[:, 0:128], lhsT=lrep[:, h, :], rhs=triu[:], start=True, stop=True),
                         reads=[lrep_r, cons_r], writes=[pB_r])
                    yield
                    P.op("dve", lambda e, H=H, pB=pB: e.tensor_tensor(out=H["argm"][:], in0=pB[:, 0:128], in1=mneg[:], op=ALU.add),
                         reads=[pB_r, cons_r], writes=[H["r"]])
                    yield
                    P.op("act", lambda e, H=H, h=h: e.activation(out=H["dec"][:], in_=H["argm"][:], func=AF.Exp, bias=gts[:, 80 + h:81 + h]),
                         reads=[H["r"], gts_r], writes=[H["r"]])
                    yield
                    P.op("act", lambda e, H=H, pB=pB, r0=r0: e.activation(out=H["eb"][r0:r0 + 64, :], in_=pB[r0:r0 + 64, 0:128], func=AF.Exp),
                         reads=[pB_r], writes=[H["r"]])
                    hfree.append(ipB)
                    yield
                    P.op("dve", lambda e, H=H, r0=r0, hh_=hh_, h=h: e.tensor_tensor(
                        out=qsT[r0:r0 + 64, hh_, :], in0=qT[r0:r0 + 64, hh_, :], in1=H["eb"][r0:r0 + 64, :], op=ALU.mult),
                        reads=[H["r"], qk_r], writes=[qs_r[h]])
                    while not hfree:
                        yield
                    ipS = hfree.pop(0)
                    pS, pS_r = gp[ipS], gp_r[ipS]
                    yield
                    P.op("pe", lambda e, pS=pS, r0=r0, hh_=hh_: e.matmul(
                        pS[:, 0:128], lhsT=kT[r0:r0 + 64, hh_, :], rhs=qT[r0:r0 + 64, hh_, :], start=True, stop=True),
                        reads=[qk_r], writes=[pS_r])
                    yield
                    P.op("dve", lambda e, H=H, pS=pS: e.tensor_tensor(out=H["sc"][:], in0=pS[:, 0:128], in1=H["dec"][:], op=ALU.mult),
                         reads=[pS_r, H["r"]], writes=[H["r"]])
                    hfree.append(ipS)
                    while not hfree:
                        yield
                    ipN = hfree.pop(0)
                    pN, pN_r = gp[ipN], gp_r[ipN]

                    def mmn(e, pN=pN, H=H, h=h, r0=r0, hh_=hh_):
                        e.matmul(pN[:, 0:128], lhsT=Cbf[r0:r0 + 64, hh_, 0:128], rhs=qsT[r0:r0 + 64, hh_, :], start=True, stop=False)
                        e.matmul(pN[:, 0:128], lhsT=vaug[:, h, 0:128], rhs=H["sc"][:], start=False, stop=True)
                        e.matmul(pN[:, 128:256], lhsT=nrep[r0:r0 + 64, hh_, :], rhs=qsT[r0:r0 + 64, hh_, :], start=True, stop=False)
                        return e.matmul(pN[:, 128:256], lhsT=ones_b[:], rhs=H["sc"][:], start=False, stop=True)
                    yield
                    P.op("pe", mmn, reads=[Cbf_r, qs_r[h], vaug_r, H["r"], cons_r], writes=[pN_r])
                    yield
                    P.op("dve", lambda e, H=H, pN=pN: e.tensor_scalar(out=H["dn"][:], in0=pN[:, 128:256], scalar1=1.0, scalar2=None,
                                                                      op0=ALU.max),
                         reads=[pN_r], writes=[H["r"]])
                    yield
                    P.op("dve", lambda e, H=H, pN=pN: e.scalar_tensor_tensor(out=H["dn"][:], in0=pN[:, 128:256], scalar=-1.0, in1=H["dn"][:],
                                                                             op0=ALU.mult, op1=ALU.max),
                         reads=[pN_r, H["r"]], writes=[H["r"]])
                    yield
                    P.op("dve", lambda e, H=H: e.reciprocal(out=H["dn"][:], in_=H["dn"][:]), reads=[H["r"]], writes=[H["r"]])
                    yield
                    P.op("dve", lambda e, H=H, pN=pN: e.tensor_tensor(out=H["hh"][:], in0=pN[:, 0:128], in1=H["dn"][:], op=ALU.mult),
                         reads=[pN_r, H["r"]], writes=[H["r"]])
                    hfree.append(ipN)
                    yield
                    P.op("pool", lambda e, H=H: e.tensor_tensor(out=H["sq"][:], in0=H["hh"][:], in1=H["hh"][:], op=ALU.mult),
                         reads=[H["r"]], writes=[H["r"]])
                    while not hfree:
                        yield
                    ipR = hfree.pop(0)
                    pR, pR_r = gp[ipR], gp_r[ipR]
                    yield
                    P.op("pe", lambda e, H=H, pR=pR: e.matmul(pR[:, 0:128], lhsT=ones_b[:], rhs=H["sq"][:], start=True, stop=True),
                         reads=[H["r"], cons_r], writes=[pR_r])
                    yield
                    P.op("act", lambda e, H=H, pR=pR: e.activation(out=H["rs"][:], in_=pR[:, 0:128], func=AF.Ln, scale=1.0 / 128,
                                                                   bias=epst[:, 0:1]),
                         reads=[pR_r, cons_r], writes=[H["r"]])
                    hfree.append(ipR)
                    yield
                    P.op("act", lambda e, H=H: e.activation(out=H["rs"][:], in_=H["rs"][:], func=AF.Exp, scale=-0.5),
                         reads=[H["r"]], writes=[H["r"]])
                    yield
                    P.op("dve", lambda e, H=H: e.tensor_tensor(out=H["hh"][:], in0=H["hh"][:], in1=H["rs"][:], op=ALU.mult),
                         reads=[H["r"]], writes=[H["r"]])
                    yield
                    P.op("dve", lambda e, H=H, h=h: e.scalar_tensor_tensor(
                        out=gT[:, h, :], in0=H["hh"][:], scalar=smallw[:, 16 + h:17 + h], in1=sgT[:, h, :], op0=ALU.mult, op1=ALU.mult),
                        reads=[H["r"], smallw_r, uT_r], writes=[big_r])
                hfree = list(range(NG))
                for h0 in range(0, 8, 8):
                    gens = [head(h0 + k, hd[k]) for k in range(8)]
                    while gens:
                        for g in list(gens):
                            try:
                                next(g)
                            except StopIteration:
                                gens.remove(g)
                for hh_ in range(4):
                    pC, pC_r = bank()

                    def mmcl(e, pC=pC, hh_=hh_):
                        e.matmul(pC[:, 0:129], lhsT=kw[:, 2 * hh_:2 * hh_ + 2, :].rearrange("p h d -> p (h d)"),
                                 rhs=vaug[:, 2 * hh_, :], start=True, stop=True)
                        return e.matmul(pC[:, 129:258], lhsT=kw[:, 2 * hh_:2 * hh_ + 2, :].rearrange("p h d -> p (h d)"),
                                        rhs=vaug[:, 2 * hh_ + 1, :], start=True, stop=True)
                    P.op("pe", mmcl, reads=[kw_r, vaug_r], writes=[pC_r])
                    for par in range(2):
                        r0 = par * 64
                        h = 2 * hh_ + par
                        P.op("dve", lambda e, pC=pC, hh_=hh_, r0=r0, par=par, h=h: e.scalar_tensor_tensor(
                            out=Cst[r0:r0 + 64, hh_, :], in0=Cst[r0:r0 + 64, hh_, :], scalar=gts[r0:r0 + 64, 88 + h:89 + h],
                            in1=pC[r0:r0 + 64, par * 129:(par + 1) * 129], op0=ALU.mult, op1=ALU.add),
                            reads=[pC_r, gts_r, Cst_r, Cbf_r], writes=[Cst_r])
                P.op("act", lambda e: e.activation(out=Cbf[:].rearrange("p c n -> p (c n)"), in_=Cst[:].rearrange("p c n -> p (c n)"), func=AF.Identity),
                     reads=[Cst_r], writes=[Cbf_r])
                for hh_ in range(4):
                    P.op("pool", lambda e, hh_=hh_: e.tensor_scalar(out=nrep[:, hh_, :], in0=ones_f[:], scalar1=Cst[:, hh_, 128:129],
                                                                    scalar2=None, op0=ALU.mult),
                         reads=[Cst_r, cons_r], writes=[Cbf_r])

                def mm2(e):
                    for n in range(2):
                        for cc in range(8):
                            r = e.matmul(psY[:, n * 512:(n + 1) * 512], lhsT=gT[:, cc, :], rhs=Wout[:, cc, n * 512:(n + 1) * 512],
                                         start=(cc == 0), stop=(cc == 7))
                    return r
                P.op("pe", mm2, reads=[W_r[1], big_r], writes=[psY_r])

            def seq_init(b):
                P.op("dve", lambda e: e.memset(vaug[:], 1.0), writes=[vaug_r])
                P.op("dve", lambda e: e.memset(Cst[:], 0.0), writes=[Cst_r])
                P.op("dve", lambda e: e.memset(Cbf[:], 0.0), writes=[Cbf_r])
                P.op("dve", lambda e: e.memset(nrep[:], 0.0), writes=[Cbf_r])
            drive(si, i, "mix", middle, seq_init)

        for si, (i, which) in enumerate(subs):
            P.barrier(bar_fns, bar_res)
            if which == "ffn":
                ffn(si, i)
            elif i % 2 == 0:
                mlstm(si, i)
            else:
                sgu(si, i)
        P.op("sp", lambda e: e.nop(), reads=dram_r + xt_r)

        with nc.Block() as block:
            P.emit(nc, block, sems)
    return nc


def _consts():
    l = np.arange(128)
    ident = np.eye(128, dtype=np.float32)
    triu = (l[:, None] <= l[None, :]).astype(np.float32)
    mneg = np.where(l[:, None] <= l[None, :], 0.0, -30000.0).astype(np.float32)
    return ident, triu, mneg


def make_in_maps(inputs, S, n_cores):
    f = lambda a: np.ascontiguousarray(np.asarray(a, dtype=np.float32))
    x = f(inputs["x"])
    c = f(inputs["c"])
    ident, triu, mneg = _consts()
    ng = f(inputs["norm_g"])
    shared = {
        "ngT": f(ng.reshape(DEPTH, 4, KD, 128).transpose(3, 0, 1, 2)),
        "norm_g": ng,
        "ada_w": f(inputs["ada_w"]), "ada_b": f(inputs["ada_b"]),
        "ffn_w1": f(inputs["ffn_w1"]), "ffn_w2": f(inputs["ffn_w2"]),
        "a_w_in": f(inputs["a_w_in"]), "a_b_if": f(inputs["a_b_if"]),
        "a_hgT": f(f(inputs["a_hnorm_g"]).reshape(2, 8, 128).transpose(0, 2, 1)),
        "a_w_out": f(inputs["a_w_out"]),
        "b_w_in": f(inputs["b_w_in"]),
        "b_binT": f(f(inputs["b_b_in"])[:, 0:2048].reshape(2, 16, 128).transpose(0, 2, 1)),
        "b_b_in": f(inputs["b_b_in"]),
        "b_ln_g": f(inputs["b_ln_g"]), "b_ln_b": f(inputs["b_ln_b"]),
        "b_wsT": f(f(inputs["b_ws"]).transpose(0, 3, 1, 2)),
        "b_bs": f(f(inputs["b_bs"]).reshape(2, 1, 2048)),
        "b_w_out": f(inputs["b_w_out"]),
        "c_ident": ident, "c_triu": triu, "c_mneg": mneg,
    }
    maps = []
    for ci in range(n_cores):
        m = dict(shared)
        m["x"] = f(x[ci * NB:(ci + 1) * NB, :S].reshape(NB * S, D))
        m["cT"] = f(c[ci * NB:(ci + 1) * NB].reshape(NB, KD, 128).transpose(2, 1, 0))
        maps.append(m)
    return maps


def run(inputs, S, subs, n_cores):
    nc = build_program(S, subs)
    maps = make_in_maps(inputs, S, n_cores)
    res = run_bass_kernel_spmd(nc, maps, core_ids=list(range(n_cores)))
    outs = [r["out"].reshape(NB, S, D) for r in res.results]
    return np.concatenate(outs, axis=0)


def kernel(**inputs):
    out = run(inputs, 2048, ALL_SUBS, 8)
    return out.astype(np.float32)
```

```python
import numpy as np
import concourse.bass as bass
import concourse.mybir as mybir
from concourse.bass_utils import run_bass_kernel_spmd
from contextlib import ExitStack

F32 = mybir.dt.float32
BF16 = mybir.dt.bfloat16
AF = mybir.ActivationFunctionType
ALU = mybir.AluOpType
AX = mybir.AxisListType

D = 1024
KD = 8
NB = 2
DEPTH = 4
EPS = 1e-6
A_IN = 3088
ALL_SUBS = [(i, w) for i in range(DEPTH) for w in ("mix", "ffn")]


class Res:
    __slots__ = ("name", "lw", "rd")

    def __init__(self, name):
        self.name = name
        self.lw = None
        self.rd = []


class DSem:
    __slots__ = ("sem", "count")

    def __init__(self, sem):
        self.sem = sem
        self.count = 0


class Op:
    __slots__ = ("eng", "fn", "deps", "dma", "sig", "val", "signaled", "cnt")


class Prog:
    ENGS = ("pe", "act", "dve", "pool", "sp")

    def __init__(self):
        self.ops = []

    def op(self, eng, fn, reads=(), writes=(), dma=0, sig=None):
        i = len(self.ops)
        deps = set()
        for r in reads:
            if r.lw is not None:
                deps.add(r.lw)
        for w in writes:
            if w.lw is not None:
                deps.add(w.lw)
            deps.update(w.rd)
        deps.discard(i)
        o = Op()
        o.eng, o.fn, o.deps, o.dma, o.sig = eng, fn, deps, dma, sig
        o.signaled = False
        o.cnt = 0
        o.val = 0
        if dma:
            sig.count += 16 * dma
            o.val = sig.count
        for r in reads:
            r.rd.append(i)
        for w in writes:
            w.lw = i
            w.rd = []
        self.ops.append(o)
        return i

    def barrier(self, fns, res):
        n0 = getattr(self, "_last_bar", 0)
        dmas = [k for k in range(n0, len(self.ops)) if self.ops[k].dma]
        for e in ("pe", "act", "dve", "pool", "sp"):
            k = self.op(e, fns[e], writes=[res[e]])
            if e == "sp":
                self.ops[k].deps.update(dmas)
        for e in ("pe", "act", "dve", "pool", "sp"):
            self.op(e, fns[e], reads=[res[x] for x in res], writes=[res[e]])
        self._last_bar = len(self.ops)

    def emit(self, nc, block, sems):
        ops = self.ops
        for o in ops:
            for d in o.deps:
                p = ops[d]
                if p.dma:
                    continue
                if p.eng == o.eng and o.eng == "pe" and not o.dma:
                    continue
                p.signaled = True
        cnts = {e: 0 for e in self.ENGS}
        for o in ops:
            if o.signaled and not o.dma:
                cnts[o.eng] += 1
                o.cnt = cnts[o.eng]
        per = {e: [] for e in self.ENGS}
        for o in ops:
            per[o.eng].append(o)

        def run(engname):
            def body(e):
                waited = {}
                for o in per[engname]:
                    need = {}
                    for d in o.deps:
                        p = ops[d]
                        if p.dma:
                            key, val, s = id(p.sig), p.val, p.sig.sem
                        else:
                            if p.eng == engname and engname == "pe" and not o.dma:
                                continue
                            key, val, s = p.eng, p.cnt, sems[p.eng]
                        if need.get(key, (0, None))[0] < val:
                            need[key] = (val, s)
                    for key, (val, s) in need.items():
                        if waited.get(key, 0) < val:
                            e.wait_ge(s, val)
                            waited[key] = val
                    r = o.fn(e)
                    if o.dma:
                        assert len(r) == o.dma
                        for ins in r:
                            ins.then_inc(o.sig.sem, 16)
                    elif o.signaled:
                        r.then_inc(sems[engname], 1)
            return body

        block.tensor(run("pe"))
        block.scalar(run("act"))
        block.vector(run("dve"))
        block.gpsimd(run("pool"))
        block.sync(run("sp"))


def build_program(S, subs, debug_mod=False):
    NC = S // 128
    TOK = NB * S
    nc = bass.Bass("TRN2", target_bir_lowering=False)
    P = Prog()

    def din(name, shape):
        return nc.dram_tensor(name, list(shape), F32, kind="ExternalInput").ap()

    x_d = din("x", [TOK, D])
    cT_d = din("cT", [128, KD, NB])
    ngT_d = din("ngT", [128, DEPTH, 4, KD])
    ng_d = din("norm_g", [DEPTH, 4, D])
    adaw_d = din("ada_w", [DEPTH, D, 6 * D])
    adab_d = din("ada_b", [DEPTH, 6 * D])
    w1_d = din("ffn_w1", [DEPTH, D, 4 * D])
    w2_d = din("ffn_w2", [DEPTH, 4 * D, D])
    awin_d = din("a_w_in", [2, D, A_IN])
    abif_d = din("a_b_if", [2, 16])
    ahgT_d = din("a_hgT", [2, 128, 8])
    awout_d = din("a_w_out", [2, D, D])
    bwin_d = din("b_w_in", [2, D, 4 * D])
    bbinT_d = din("b_binT", [2, 128, 16])
    bbin_d = din("b_b_in", [2, 4 * D])
    blng_d = din("b_ln_g", [2, 2048])
    blnb_d = din("b_ln_b", [2, 2048])
    bwsT_d = din("b_wsT", [2, 128, 16, 128])
    bbs_d = din("b_bs", [2, 1, 2048])
    bwout_d = din("b_w_out", [2, 2048, D])
    ident_d = din("c_ident", [128, 128])
    triu_d = din("c_triu", [128, 128])
    mneg_d = din("c_mneg", [128, 128])
    out_d = nc.dram_tensor("out", [TOK, D], F32, kind="ExternalOutput").ap()
    xs_d = nc.dram_tensor("xs", [TOK, D], F32, kind="Internal").ap()
    mod_d = nc.dram_tensor("mod_d", [DEPTH, NB, 6 * D], F32, kind="Internal").ap()

    es = ExitStack()
    with es:
        def sb(name, shape, dt=F32):
            return es.enter_context(nc.sbuf_tensor(name, list(shape), dt))

        def pst(name, shape, dt=F32):
            return es.enter_context(nc.psum_tensor(name, list(shape), dt))

        def sem(name):
            return es.enter_context(nc.semaphore(name))

        sems = {e: sem("s_" + e) for e in ("pe", "act", "dve", "pool", "sp")}

        def dsem(name):
            return DSem(sem("d_" + name))

        wbuf = sb("wbuf", [128, 65536], BF16)
        NXS = 3
        xt = [sb(f"xt{i}", [128, D]) for i in range(NXS)]
        xt_r = [Res(f"xt{i}") for i in range(NXS)]
        xt_s = [dsem(f"xt{i}") for i in range(NXS)]
        xn = sb("xn", [128, D], BF16)
        xn_r = Res("xn")
        hT = [sb(f"hT{i}", [128, KD, 128], BF16) for i in range(2)]
        hT_r = [Res(f"hT{i}") for i in range(2)]
        big = sb("big", [128, 4096], BF16)
        big_r = Res("big")
        tt = sb("tt", [128, D])
        tt_r = Res("tt")
        tj, tj_r = tt, tt_r
        rbuf = [sb(f"rb{i}", [128, 512]) for i in range(2)]
        rbuf_r = [Res(f"rb{i}") for i in range(2)]
        gbc = sb("gbc", [128, D])
        gbc_r = Res("gbc")
        gbc_s = dsem("gbc")
        ngbc, ngbc_r = tt, tt_r
        ngbc_s = dsem("ngbc")
        st = sb("st", [128, 32])
        st_r = Res("st")
        stt_ = sb("stt_", [128, 24])
        modT = sb("modT", [128, DEPTH, NB, 6, KD])
        modT_r = Res("modT")
        modT_s = dsem("modT")
        ASb = sb("AS", [128, DEPTH, 4, KD, NB])
        AS_r = Res("AS")
        ngT = sb("ngTs", [128, DEPTH, 4, KD])
        cons_r = Res("cons")
        cons_s = dsem("cons")
        cact = sb("cact", [128, KD, NB])
        identb = sb("identb", [128, 128], BF16)
        triu = sb("triu", [128, 128])
        mneg = sb("mneg", [128, 128])
        ones_f = sb("ones_f", [128, 128])
        ones_b = sb("ones_b", [128, 128], BF16)
        epst = sb("epst", [128, 1])
        one1 = sb("one1", [128, 1])
        adab = wbuf[0:NB, 32768:32768 + 12288].bitcast(F32)
        modsb = wbuf[0:NB, 45056:45056 + 12288].bitcast(F32)
        modsb_r = Res("modsb")
        modsb_s = dsem("modsb")
        mixbuf = sb("mixbuf", [128, 18432], BF16)

        def carver():
            cur = [0]

            def carve(name, shape, dt=F32):
                n = int(np.prod(shape[1:]))
                nb = n * (4 if dt == F32 else 2)
                a = cur[0]
                cur[0] += ((nb + 63) // 64 * 64) // 2
                assert cur[0] <= 18432, name
                v = mixbuf[:, a:a + nb // 2]
                if dt == F32:
                    v = v.bitcast(F32)
                if len(shape) == 3:
                    v = v.rearrange("p (a b) -> p a b", a=shape[1])
                return v
            return carve
        sbs = carver()
        sbm = carver()
        v32 = sbs("v32", [128, 2048])
        v32_r = Res("v32")
        vnb = sbs("vnb", [128, 2048], BF16)
        vnb_r = Res("vnb")
        uT = sbs("uT", [128, 16, 128])
        sgT_m = sbm("sgT", [128, 8, 128])
        uT_r = Res("uT")
        lng = sbs("lng", [128, 2048])
        lnb = sbs("lnb", [128, 2048])
        lnp_r = Res("lnp")
        lnp_s = dsem("lnp")
        smallw = sb("smallw", [128, 64])
        smallw_r = Res("smallw")
        smallw_s = dsem("smallw")
        rowb = wbuf[0:1, 51200:57344]
        rowf = wbuf[0:1, 57344:65536].bitcast(F32)
        row_s = dsem("row")
        qT = sbm("qT", [128, 4, 128], BF16)
        kT = sbm("kT", [128, 4, 128], BF16)
        qsT = sbm("qsT", [128, 4, 128], BF16)
        qk_r = Res("qk")
        qs_r = [Res(f"qs{h}") for h in range(8)]
        kw = sbm("kw", [128, 8, 64], BF16)
        kw_r = Res("kw")
        vaug = sbm("vaug", [128, 8, 129], BF16)
        vaug_r = Res("vaug")
        gts = sbm("gts", [128, 128])
        gts_r = Res("gts")
        lrep = sbm("lrep", [128, 8, 128])
        lrep_r = Res("lrep")
        Cst = sbm("Cst", [128, 4, 129])
        Cst_r = Res("Cst")
        Cbf = sbm("Cbf", [128, 4, 129], BF16)
        nrep = sbm("nrep", [128, 4, 128], BF16)
        Cbf_r = Res("Cbf")
        hd = []
        for i in range(8):
            _a, _d, _e = sbm(f"argm{i}", [128, 128]), sbm(f"dec{i}", [128, 128]), sbm(f"ebh{i}", [128, 128])
            hd.append(dict(argm=_a, dec=_d, eb=_e, sc=sbm(f"sc{i}", [128, 128], BF16), dn=_a, hh=_d,
                           sq=sbm(f"sq{i}", [128, 128], BF16), rs=_e, r=Res(f"hd{i}")))
        wtmp = v32[:].rearrange("p (g t) -> p g t", g=16)
        wtmp_r = v32_r
        wtmp_s = dsem("wtmp")

        psT = pst("psT", [128, 1024], BF16)
        psT_r = Res("psT")
        psY = pst("psY", [128, 1024])
        psY_r = Res("psY")
        NG = 5
        gp = [pst(f"gp{i}", [128, 512]) for i in range(NG)]
        gp_r = [Res(f"gp{i}") for i in range(NG)]
        gctr = [0]

        def bank():
            i = gctr[0] % NG
            gctr[0] += 1
            return gp[i], gp_r[i]

        W_r = [Res("W0"), Res("W1")]
        W_s = [dsem("W0"), dsem("W1")]

        bar_t = sb("bar_t", [128, 8])
        bar_res = {e: Res("bar_" + e) for e in ("pe", "act", "dve", "pool", "sp")}
        bar_fns = {
            "pe": lambda e: e.matmul(psY[0:1, 0:2], lhsT=identb[:, 0:1], rhs=identb[:, 0:2], start=True, stop=True),
            "act": lambda e: e.activation(out=bar_t[:, 0:1], in_=epst[:, 0:1], func=AF.Identity),
            "dve": lambda e: e.memset(bar_t[:, 2:3], 0.0),
            "pool": lambda e: e.memset(bar_t[:, 4:5], 0.0),
            "sp": lambda e: e.nop(),
        }
        bar_res["pe"] = psY_r
        def load_consts(e):
            return [
                e.dma_start(out=cact[:], in_=cT_d),
                e.dma_start(out=ngT[:], in_=ngT_d),
                e.dma_start(out=triu[:], in_=triu_d),
                e.dma_start(out=mneg[:], in_=mneg_d),
            ]
        P.op("sp", load_consts, writes=[cons_r], dma=4, sig=cons_s)
        P.op("pool", lambda e: [e.dma_start(out=identb[:], in_=ident_d)], writes=[cons_r], dma=1, sig=cons_s)
        P.op("dve", lambda e: e.memset(ones_f[:], 1.0), writes=[cons_r])
        P.op("dve", lambda e: e.memset(ones_b[:], 1.0), writes=[cons_r])
        P.op("dve", lambda e: e.memset(epst[:], EPS), writes=[cons_r])
        P.op("dve", lambda e: e.memset(one1[:], 1.0), writes=[cons_r])
        P.op("act", lambda e: e.activation(out=cact[:], in_=cact[:], func=AF.Silu), reads=[cons_r], writes=[cons_r])

        layers_used = sorted(set(i for i, _ in subs))
        adaslot = [wbuf[:, s * 8192:(s + 1) * 8192].bitcast(F32).rearrange("p (k n) -> p k n", k=KD) for s in range(3)]
        ada_r = [Res(f"ada{s}") for s in range(3)]
        ada_s = [dsem(f"ada{s}") for s in range(3)]
        blk = 0
        for i in layers_used:
            def ld_adab(e, i=i):
                return [e.dma_start(out=adab[:], in_=adab_d[i:i + 1, :].partition_broadcast(NB))]
            P.op("sp", ld_adab, writes=[modsb_r], dma=1, sig=modsb_s)
            for j in range(12):
                s = blk % 3
                blk += 1

                def ld(e, i=i, j=j, s=s):
                    return [e.dma_start(out=adaslot[s][:, k, :], in_=adaw_d[i, k * 128:(k + 1) * 128, j * 512:(j + 1) * 512])
                            for k in range(KD)]
                P.op("sp", ld, writes=[ada_r[s]], dma=KD, sig=ada_s[s])
                pb, pb_r = bank()

                def mm(e, s=s, pb=pb):
                    for k in range(KD):
                        r = e.matmul(pb[0:NB, :], lhsT=cact[:, k, :], rhs=adaslot[s][:, k, :], start=(k == 0), stop=(k == KD - 1))
                    return r
                P.op("pe", mm, reads=[ada_r[s], cons_r], writes=[pb_r])
                P.op("dve", lambda e, j=j, pb=pb: e.tensor_tensor(out=modsb[:, j * 512:(j + 1) * 512], in0=pb[0:NB, :],
                                                                  in1=adab[:, j * 512:(j + 1) * 512], op=ALU.add),
                     reads=[pb_r], writes=[modsb_r])
            P.op("sp", lambda e, i=i: [e.dma_start(out=mod_d[i], in_=modsb[:])], reads=[modsb_r], writes=[modT_r],
                 dma=1, sig=modsb_s)
            for b in range(NB):
                def ldT(e, i=i, b=b):
                    return [e.dma_start(out=modT[:, i, b, v, :],
                                        in_=mod_d[i, b, v * D:(v + 1) * D].rearrange("(k p) -> p k", p=128),
                                        allow_slow_non_contiguous=True) for v in range(6)]
                P.op("sp", ldT, reads=[modT_r], writes=[modT_r], dma=6, sig=modT_s)
            for b in range(NB):
                for (w, vsc, vsh, j) in ((0, 1, 0, 0), (2, 4, 3, 2)):
                    P.op("dve", lambda e, i=i, b=b, w=w, vsc=vsc, j=j: e.scalar_tensor_tensor(
                        out=ASb[:, i, w, :, b], in0=modT[:, i, b, vsc, :], scalar=1.0, in1=ngT[:, i, j, :],
                        op0=ALU.add, op1=ALU.mult), reads=[modT_r, cons_r], writes=[AS_r])
                    P.op("dve", lambda e, i=i, b=b, w=w, vsh=vsh: e.tensor_copy(out=ASb[:, i, w + 1, :, b], in_=modT[:, i, b, vsh, :]),
                         reads=[modT_r], writes=[AS_r])
        for s in range(3):
            W_r[0].rd += ada_r[s].rd
            if ada_r[s].lw is not None:
                W_r[0].rd.append(ada_r[s].lw)
        W_r[1].rd += modsb_r.rd
        if modsb_r.lw is not None:
            W_r[1].rd.append(modsb_r.lw)

        xctr = [0]
        hctr = [0]

        def src_dst(si):
            src = x_d if si == 0 else xs_d
            dst = out_d if si == len(subs) - 1 else xs_d
            return src, dst
        dram_r = [Res(f"rows{t}") for t in range(NB * NC)]

        stf_r = [Res("stf0"), Res("stf1"), Res("stf2")]
        stb_r = Res("stb")

        def front_a(si, i, which, b, t):
            src, _ = src_dst(si)
            xs_ = xctr[0] % NXS
            xctr[0] += 1
            st_r = stf_r[xs_]
            st = stt_[:, 8 * xs_:8 * xs_ + 8]
            P.op("sp", lambda e: [e.dma_start(out=xt[xs_][:], in_=src[t * 128:(t + 1) * 128, :])],
                 reads=[dram_r[t]], writes=[xt_r[xs_]], dma=1, sig=xt_s[xs_])
            P.op("act", lambda e: e.activation(out=xn[:], in_=xt[xs_][:], func=AF.Square, accum_out=st[:, 0:1]),
                 reads=[xt_r[xs_]], writes=[xn_r, st_r])
            P.op("act", lambda e: e.activation(out=st[:, 1:2], in_=st[:, 0:1], func=AF.Sqrt, scale=1.0 / D, bias=epst[:, 0:1]),
                 reads=[st_r, cons_r], writes=[st_r])
            P.op("dve", lambda e: e.reciprocal(out=st[:, 2:3], in_=st[:, 1:2]), reads=[st_r], writes=[st_r])
            P.op("dve", lambda e: e.tensor_scalar(out=xn[:], in0=xt[xs_][:], scalar1=st[:, 2:3], scalar2=None, op0=ALU.mult),
                 reads=[xt_r[xs_], st_r], writes=[xn_r])
            return xs_

        def front_b(i, which, b):
            hs_ = hctr[0] % 2
            hctr[0] += 1
            w = 0 if which == "mix" else 2

            def tr(e):
                for k in range(KD):
                    r = e.transpose(out=psT[:, k * 128:(k + 1) * 128], in_=xn[:, k * 128:(k + 1) * 128], identity=identb[:])
                return r
            P.op("pe", tr, reads=[xn_r, cons_r], writes=[psT_r])
            for k in range(KD):
                if k % 2 == 0:
                    P.op("act", lambda e, k=k: e.activation(out=hT[hs_][:, k, :], in_=psT[:, k * 128:(k + 1) * 128], func=AF.Identity,
                                                            scale=ASb[:, i, w, k, b:b + 1], bias=ASb[:, i, w + 1, k, b:b + 1]),
                         reads=[psT_r, AS_r], writes=[hT_r[hs_]])
                else:
                    P.op("dve", lambda e, k=k: e.tensor_scalar(out=hT[hs_][:, k, :], in0=psT[:, k * 128:(k + 1) * 128],
                                                               scalar1=ASb[:, i, w, k, b:b + 1], scalar2=ASb[:, i, w + 1, k, b:b + 1],
                                                               op0=ALU.mult, op1=ALU.add),
                         reads=[psT_r, AS_r], writes=[hT_r[hs_]])
            return hs_

        def drive(si, i, which, middle, seq_init=None):
            seq = [(b, c) for b in range(NB) for c in range(NC)]
            slots = {}
            slots[0] = [front_a(si, i, which, 0, 0), None]
            slots[0][1] = front_b(i, which, 0)
            for idx, (b, c) in enumerate(seq):
                t = b * NC + c
                if c == 0:
                    load_gate(i, which, b)
                    if seq_init is not None:
                        seq_init(b)
                nxt = seq[idx + 1] if idx + 1 < len(seq) else None
                if nxt is not None:
                    slots[t + 1] = [front_a(si, i, which, nxt[0], t + 1), None]
                middle(b, c, t, slots[t][0], slots[t][1])
                if nxt is not None:
                    slots[t + 1][1] = front_b(i, which, nxt[0])
                back(si, t, slots[t][0])

        def back(si, t, xs_):
            _, dst = src_dst(si)
            P.op("act", lambda e: e.activation(out=tj[:], in_=psY[:], func=AF.Square, accum_out=st[:, 4:5]),
                 reads=[psY_r], writes=[tj_r, stb_r])
            P.op("act", lambda e: e.activation(out=st[:, 5:6], in_=st[:, 4:5], func=AF.Sqrt, scale=1.0 / D, bias=epst[:, 0:1]),
                 reads=[stb_r, cons_r], writes=[stb_r])
            P.op("dve", lambda e: e.reciprocal(out=st[:, 6:7], in_=st[:, 5:6]), reads=[stb_r], writes=[stb_r])
            P.op("dve", lambda e: e.scalar_tensor_tensor(out=tt[:], in0=psY[:], scalar=st[:, 6:7], in1=gbc[:], op0=ALU.mult, op1=ALU.mult),
                 reads=[psY_r, stb_r, gbc_r], writes=[tt_r])
            P.op("pool", lambda e: e.tensor_tensor(out=xt[xs_][:], in0=xt[xs_][:], in1=tt[:], op=ALU.add),
                 reads=[tt_r, xt_r[xs_]], writes=[xt_r[xs_]])
            P.op("sp", lambda e: [e.dma_start(out=dst[t * 128:(t + 1) * 128, :], in_=xt[xs_][:])],
                 reads=[xt_r[xs_]], writes=[dram_r[t]], dma=1, sig=xt_s[xs_])

        def load_gate(i, which, b):
            v = 2 if which == "mix" else 5
            j = 1 if which == "mix" else 3
            P.op("sp", lambda e: [e.dma_start(out=gbc[:], in_=mod_d[i, b:b + 1, v * D:(v + 1) * D].partition_broadcast(128))],
                 reads=[modT_r], writes=[gbc_r], dma=1, sig=gbc_s)
            P.op("sp", lambda e: [e.dma_start(out=ngbc[:], in_=ng_d[i, j:j + 1, :].partition_broadcast(128))],
                 writes=[ngbc_r], dma=1, sig=ngbc_s)
            P.op("dve", lambda e: e.tensor_tensor(out=gbc[:], in0=gbc[:], in1=ngbc[:], op=ALU.mult),
                 reads=[gbc_r, ngbc_r], writes=[gbc_r])

        def wview(off, k, n):
            return wbuf[:, off:off + k * n].rearrange("p (k n) -> p k n", k=k)

        def ffn(si, i):
            W1 = wview(0, KD, 4096)
            W2 = wview(32768, 32, D)
            P.op("pool", lambda e: [e.dma_start(out=W1[:, k, :], in_=w1_d[i, k * 128:(k + 1) * 128, :]) for k in range(KD)],
                 writes=[W_r[0]], dma=KD, sig=W_s[0])
            P.op("pool", lambda e: [e.dma_start(out=W2[:, 4 * k:4 * k + 4, :],
                                                in_=w2_d[i, k * 512:(k + 1) * 512, :].rearrange("(c p) n -> p c n", p=128))
                                    for k in range(8)],
                 writes=[W_r[1]], dma=8, sig=W_s[1])
            hidT = big[:].rearrange("p (c n) -> p c n", c=32)
            def middle(b, c, t, xs_, hs_):
                for q in range(8):
                    pb, pb_r = bank()

                    def mm(e, q=q, pb=pb):
                        for cc in range(4):
                            hc = q * 4 + cc
                            for k in range(KD):
                                r = e.matmul(pb[:, cc * 128:(cc + 1) * 128], lhsT=W1[:, k, hc * 128:(hc + 1) * 128],
                                             rhs=hT[hs_][:, k, :], start=(k == 0), stop=(k == KD - 1))
                        return r
                    P.op("pe", mm, reads=[W_r[0], hT_r[hs_]], writes=[pb_r])
                    rs_ = q % 2
                    P.op("act", lambda e, pb=pb, rs_=rs_: e.activation(out=rbuf[rs_][:], in_=pb[:], func=AF.Relu),
                         reads=[pb_r], writes=[rbuf_r[rs_]])
                    P.op("pool", lambda e, q=q, rs_=rs_: e.tensor_tensor(
                        out=big[:, q * 512:(q + 1) * 512], in0=rbuf[rs_][:], in1=rbuf[rs_][:], op=ALU.mult),
                        reads=[rbuf_r[rs_]], writes=[big_r])

                def mm2(e):
                    for n in range(2):
                        for hc in range(32):
                            r = e.matmul(psY[:, n * 512:(n + 1) * 512], lhsT=hidT[:, hc, :], rhs=W2[:, hc, n * 512:(n + 1) * 512],
                                         start=(hc == 0), stop=(hc == 31))
                    return r
                P.op("pe", mm2, reads=[W_r[1], big_r], writes=[psY_r])
            drive(si, i, "ffn", middle)

        def sgu(si, i):
            j = i // 2
            Win = wview(0, KD, 4096)
            Wout = wview(32768, 16, D)
            wmT = wview(32768 + 16384, 16, 128)
            P.op("pool", lambda e: [e.dma_start(out=Win[:, k, :], in_=bwin_d[j, k * 128:(k + 1) * 128, :]) for k in range(KD)],
                 writes=[W_r[0]], dma=KD, sig=W_s[0])
            P.op("pool", lambda e: [e.dma_start(out=Wout[:, 4 * k:4 * k + 4, :],
                                                in_=bwout_d[j, k * 512:(k + 1) * 512, :].rearrange("(c p) n -> p c n", p=128))
                                    for k in range(4)],
                 writes=[W_r[1]], dma=4, sig=W_s[1])
            P.op("sp", lambda e: [e.dma_start(out=wtmp[:], in_=bwsT_d[j])], writes=[wtmp_r], dma=1, sig=wtmp_s)
            for g in range(16):
                P.op("pool", lambda e, g=g: e.tensor_tensor(out=wmT[:, g, :], in0=wtmp[:, g, :], in1=(triu[:]), op=ALU.mult),
                     reads=[wtmp_r, cons_r], writes=[W_r[1]])
            P.op("sp", lambda e: [e.dma_start(out=lng[:], in_=blng_d[j:j + 1, :].partition_broadcast(128)),
                                  e.dma_start(out=lnb[:], in_=blnb_d[j:j + 1, :].partition_broadcast(128))],
                 writes=[lnp_r], dma=2, sig=lnp_s)
            P.op("sp", lambda e: [e.dma_start(out=smallw[:, 0:16], in_=bbinT_d[j])], writes=[smallw_r], dma=1, sig=smallw_s)
            P.op("pool", lambda e: [e.dma_start(out=rowb[0:1, 0:2048], in_=bbin_d[j:j + 1, 2048:4096]),
                                    e.dma_start(out=rowb[0:1, 2048:4096], in_=bbs_d[j])],
                 writes=[W_r[1]], dma=2, sig=row_s)
            P.op("sp", lambda e: [e.dma_start(out=rowf[0:1, 0:2048], in_=bbs_d[j])], writes=[W_r[1]], dma=1, sig=row_s)
            P.op("dve", lambda e: e.tensor_copy(out=rowf[0:1, 2048:4096], in_=rowb[0:1, 2048:4096]), reads=[W_r[1]], writes=[W_r[1]])
            P.op("dve", lambda e: e.tensor_tensor(out=rowb[0:1, 4096:6144], in0=rowf[0:1, 0:2048], in1=rowf[0:1, 2048:4096], op=ALU.subtract),
                 reads=[W_r[1]], writes=[W_r[1]])
            yT = big[:, 0:2048].rearrange("p (c n) -> p c n", c=16)
            def middle(b, c, t, xs_, hs_):
                for n in range(4):
                    pb, pb_r = bank()

                    def mmv(e, n=n, pb=pb):
                        for k in range(KD):
                            e.matmul(pb[:], lhsT=hT[hs_][:, k, :], rhs=Win[:, k, 2048 + n * 512:2048 + (n + 1) * 512],
                                     start=(k == 0), stop=False)
                        return e.matmul(pb[:], lhsT=ones_b[0:1, :], rhs=rowb[0:1, n * 512:(n + 1) * 512], start=False, stop=True)
                    P.op("pe", mmv, reads=[W_r[0], hT_r[hs_], W_r[1], cons_r], writes=[pb_r])
                    P.op("act", lambda e, n=n, pb=pb: e.activation(out=v32[:, n * 512:(n + 1) * 512], in_=pb[:], func=AF.Gelu,
                                                                   accum_out=st[:, 8 + n:9 + n]),
                         reads=[pb_r], writes=[v32_r, st_r])
                P.op("act", lambda e: e.activation(out=vnb[:], in_=v32[:], func=AF.Square, accum_out=st[:, 12:13]),
                     reads=[v32_r], writes=[vnb_r, st_r])
                P.op("dve", lambda e: e.reduce_sum(out=st[:, 13:14], in_=st[:, 8:12], axis=AX.X), reads=[st_r], writes=[st_r])
                P.op("dve", lambda e: e.tensor_scalar(out=st[:, 14:15], in0=st[:, 13:14], scalar1=1.0 / 2048, scalar2=None, op0=ALU.mult),
                     reads=[st_r], writes=[st_r])
                P.op("dve", lambda e: e.tensor_tensor(out=st[:, 15:16], in0=st[:, 14:15], in1=st[:, 14:15], op=ALU.mult),
                     reads=[st_r], writes=[st_r])
                P.op("dve", lambda e: e.scalar_tensor_tensor(out=st[:, 16:17], in0=st[:, 12:13], scalar=1.0 / 2048, in1=st[:, 15:16],
                                                             op0=ALU.mult, op1=ALU.subtract), reads=[st_r], writes=[st_r])
                P.op("act", lambda e: e.activation(out=st[:, 17:18], in_=st[:, 16:17], func=AF.Sqrt, bias=epst[:, 0:1]),
                     reads=[st_r, cons_r], writes=[st_r])
                P.op("dve", lambda e: e.reciprocal(out=st[:, 18:19], in_=st[:, 17:18]), reads=[st_r], writes=[st_r])
                P.op("dve", lambda e: e.scalar_tensor_tensor(out=st[:, 19:20], in0=st[:, 14:15], scalar=-1.0, in1=st[:, 18:19],
                                                             op0=ALU.mult, op1=ALU.mult), reads=[st_r], writes=[st_r])
                P.op("act", lambda e: e.activation(out=v32[:], in_=v32[:], func=AF.Identity, scale=st[:, 18:19], bias=st[:, 19:20]),
                     reads=[v32_r, st_r], writes=[v32_r])
                P.op("pool", lambda e: e.tensor_tensor(out=v32[:], in0=v32[:], in1=lng[:], op=ALU.mult),
                     reads=[v32_r, lnp_r], writes=[v32_r])
                P.op("dve", lambda e: e.tensor_tensor(out=vnb[:], in0=v32[:], in1=lnb[:], op=ALU.add),
                     reads=[v32_r, lnp_r], writes=[vnb_r])
                for q in range(4):
                    pb, pb_r = bank()

                    def mm(e, q=q, pb=pb):
                        for cc in range(4):
                            uc = q * 4 + cc
                            for k in range(KD):
                                r = e.matmul(pb[:, cc * 128:(cc + 1) * 128], lhsT=Win[:, k, uc * 128:(uc + 1) * 128],
                                             rhs=hT[hs_][:, k, :], start=(k == 0), stop=(k == KD - 1))
                        return r
                    P.op("pe", mm, reads=[W_r[0], hT_r[hs_]], writes=[pb_r])
                    for cc in range(4):
                        uc = q * 4 + cc
                        P.op("act", lambda e, pb=pb, cc=cc, uc=uc: e.activation(
                            out=uT[:, uc, :], in_=pb[:, cc * 128:(cc + 1) * 128], func=AF.Gelu, bias=smallw[:, uc:uc + 1]),
                            reads=[pb_r, smallw_r], writes=[uT_r])
                for q in range(4):
                    pb, pb_r = bank()

                    def mms(e, q=q, pb=pb):
                        for cc in range(4):
                            g = q * 4 + cc
                            o = pb[:, cc * 128:(cc + 1) * 128]
                            e.matmul(o, lhsT=vnb[:, g * 128:(g + 1) * 128], rhs=wmT[:, g, :], start=True, stop=False)
                            e.matmul(o, lhsT=ones_b[0:1, :], rhs=rowb[0:1, 2048 + g * 128:2048 + (g + 1) * 128], start=False, stop=False)
                            r = e.matmul(o, lhsT=ones_b[0:1, :], rhs=rowb[0:1, 4096 + g * 128:4096 + (g + 1) * 128], start=False, stop=True)
                        return r
                    P.op("pe", mms, reads=[vnb_r, W_r[1], W_r[1], cons_r], writes=[pb_r])
                    P.op("dve", lambda e, q=q, pb=pb: e.tensor_tensor(
                        out=big[:, q * 512:(q + 1) * 512], in0=uT[:, q * 4:(q + 1) * 4, :].rearrange("p c n -> p (c n)"),
                        in1=pb[:], op=ALU.mult), reads=[uT_r, pb_r], writes=[big_r])

                def mm2(e):
                    for n in range(2):
                        for cc in range(16):
                            r = e.matmul(psY[:, n * 512:(n + 1) * 512], lhsT=yT[:, cc, :], rhs=Wout[:, cc, n * 512:(n + 1) * 512],
                                         start=(cc == 0), stop=(cc == 15))
                    return r
                P.op("pe", mm2, reads=[W_r[1], big_r], writes=[psY_r])
            drive(si, i, "mix", middle)

        def mlstm(si, i):
            j = i // 2
            Win = wview(0, KD, A_IN)
            Wout = wview(32768, KD, D)
            P.op("pool", lambda e: [e.dma_start(out=Win[:, k, :], in_=awin_d[j, k * 128:(k + 1) * 128, :]) for k in range(KD)],
                 writes=[W_r[0]], dma=KD, sig=W_s[0])
            P.op("pool", lambda e: [e.dma_start(out=Wout[:, 4 * k:4 * k + 4, :],
                                                in_=awout_d[j, k * 512:(k + 1) * 512, :].rearrange("(c p) n -> p c n", p=128))
                                    for k in range(2)],
                 writes=[W_r[1]], dma=2, sig=W_s[1])
            P.op("sp", lambda e: [e.dma_start(out=smallw[:, 16:24], in_=ahgT_d[j]),
                                  e.dma_start(out=smallw[:, 32:48], in_=abif_d[j:j + 1, :].partition_broadcast(128))],
                 writes=[smallw_r], dma=2, sig=smallw_s)
            gT = big[:, 0:1024].rearrange("p (c n) -> p c n", c=8)
            sgT = sgT_m
            def middle(b, c, t, xs_, hs_):
                pbg, pbg_r = bank()

                def mmg(e, pbg=pbg):
                    for k in range(KD):
                        r = e.matmul(pbg[:, 0:16], lhsT=hT[hs_][:, k, :], rhs=Win[:, k, 3072:3088], start=(k == 0), stop=(k == KD - 1))
                    return r
                P.op("pe", mmg, reads=[W_r[0], hT_r[hs_]], writes=[pbg_r])
                P.op("dve", lambda e, pbg=pbg: e.tensor_tensor(out=gts[:, 0:16], in0=pbg[:, 0:16], in1=smallw[:, 32:48], op=ALU.add),
                     reads=[pbg_r, smallw_r], writes=[gts_r])
                P.op("act", lambda e: e.activation(out=gts[:, 16:32], in_=gts[:, 0:16], func=AF.Tanh, scale=1.0 / 15.0),
                     reads=[gts_r], writes=[gts_r])
                P.op("dve", lambda e: e.tensor_scalar(out=gts[:, 32:40], in0=gts[:, 16:24], scalar1=15.0, scalar2=None, op0=ALU.mult),
                     reads=[gts_r], writes=[gts_r])
                P.op("act", lambda e: e.activation(out=gts[:, 40:48], in_=gts[:, 24:32], func=AF.Exp, scale=-15.0),
                     reads=[gts_r], writes=[gts_r])
                P.op("act", lambda e: e.activation(out=gts[:, 48:56], in_=gts[:, 40:48], func=AF.Ln, bias=one1[:, 0:1]),
                     reads=[gts_r, cons_r], writes=[gts_r])
                P.op("dve", lambda e: e.tensor_scalar(out=gts[:, 48:56], in0=gts[:, 48:56], scalar1=-1.0, scalar2=None, op0=ALU.mult),
                     reads=[gts_r], writes=[gts_r])
                for (col0, dstT, scl) in ((0, qT, 1.0), (512, kT, 0.125)):
                    pb, pb_r = bank()

                    def mm(e, col0=col0, pb=pb):
                        for cc in range(4):
                            for k in range(KD):
                                r = e.matmul(pb[:, cc * 128:(cc + 1) * 128], lhsT=Win[:, k, col0 + cc * 128:col0 + (cc + 1) * 128],
                                             rhs=hT[hs_][:, k, :], start=(k == 0), stop=(k == KD - 1))
                        return r
                    P.op("pe", mm, reads=[W_r[0], hT_r[hs_]], writes=[pb_r])
                    P.op("act", lambda e, pb=pb, dstT=dstT, scl=scl: e.activation(
                        out=dstT[:].rearrange("p c n -> p (c n)"), in_=pb[:], func=AF.Identity, scale=scl),
                        reads=[pb_r], writes=[qk_r])
                for q in range(2):
                    pb, pb_r = bank()

                    def mmo(e, q=q, pb=pb):
                        for cc in range(4):
                            oc = 2048 + (q * 4 + cc) * 128
                            for k in range(KD):
                                r = e.matmul(pb[:, cc * 128:(cc + 1) * 128], lhsT=Win[:, k, oc:oc + 128],
                                             rhs=hT[hs_][:, k, :], start=(k == 0), stop=(k == KD - 1))
                        return r
                    P.op("pe", mmo, reads=[W_r[0], hT_r[hs_]], writes=[pb_r])
                    P.op("act", lambda e, q=q, pb=pb: e.activation(
                        out=sgT[:, q * 4:(q + 1) * 4, :].rearrange("p c n -> p (c n)"), in_=pb[:], func=AF.Sigmoid),
                        reads=[pb_r], writes=[uT_r])
                pbk, pbk_r = bank()

                def mmk(e, pbk=pbk):
                    for k in range(KD):
                        r = e.matmul(pbk[:], lhsT=hT[hs_][:, k, :], rhs=Win[:, k, 512:1024], start=(k == 0), stop=(k == KD - 1))
                    return r
                P.op("pe", mmk, reads=[W_r[0], hT_r[hs_]], writes=[pbk_r])
                for n in range(2):
                    pb, pb_r = bank()

                    def mmv(e, n=n, pb=pb):
                        for k in range(KD):
                            r = e.matmul(pb[:], lhsT=hT[hs_][:, k, :], rhs=Win[:, k, 1024 + n * 512:1024 + (n + 1) * 512],
                                         start=(k == 0), stop=(k == KD - 1))
                        return r
                    P.op("pe", mmv, reads=[W_r[0], hT_r[hs_]], writes=[pb_r])
                    P.op("act", lambda e, n=n, pb=pb: e.activation(
                        out=vaug[:, n * 4:(n + 1) * 4, 0:128], in_=pb[:].rearrange("p (c n) -> p c n", c=4), func=AF.Identity),
                        reads=[pb_r], writes=[vaug_r])
                pbc, pbc_r = bank()

                def mmc(e, pbc=pbc):
                    e.matmul(pbc[:, 0:8], lhsT=triu[:], rhs=gts[:, 48:56], start=True, stop=True)
                    return e.matmul(pbc[:, 8:16], lhsT=ones_f[:], rhs=gts[:, 48:56], start=True, stop=True)
                P.op("pe", mmc, reads=[gts_r, cons_r], writes=[pbc_r])
                P.op("dve", lambda e, pbc=pbc: e.tensor_copy(out=gts[:, 56:72], in_=pbc[:, 0:16]), reads=[pbc_r], writes=[gts_r])
                P.op("dve", lambda e: e.tensor_tensor(out=gts[:, 80:88], in0=gts[:, 32:40], in1=gts[:, 56:64], op=ALU.subtract),
                     reads=[gts_r], writes=[gts_r])
                P.op("dve", lambda e: e.tensor_tensor(out=gts[:, 72:80], in0=gts[:, 80:88], in1=gts[:, 64:72], op=ALU.add),
                     reads=[gts_r], writes=[gts_r])
                P.op("act", lambda e: e.activation(out=gts[:, 72:80], in_=gts[:, 72:80], func=AF.Exp), reads=[gts_r], writes=[gts_r])
                P.op("act", lambda e: e.activation(out=gts[:, 88:96], in_=gts[:, 64:72], func=AF.Exp), reads=[gts_r], writes=[gts_r])
                for h in range(8):
                    P.op("dve", lambda e, pbk=pbk, h=h: e.tensor_scalar(
                        out=kw[:, h, :], in0=pbk[:, h * 64:(h + 1) * 64], scalar1=gts[:, 72 + h:73 + h], scalar2=0.125,
                        op0=ALU.mult, op1=ALU.mult), reads=[pbk_r, gts_r], writes=[kw_r])
                for h in range(8):
                    P.op("pool", lambda e, h=h: e.tensor_scalar(out=lrep[:, h, :], in0=ones_f[:], scalar1=gts[:, 48 + h:49 + h],
                                                                scalar2=None, op0=ALU.mult),
                         reads=[gts_r, cons_r], writes=[lrep_r])
                def head(h, H):
                    hh_, r0 = h // 2, (h % 2) * 64
                    while not hfree:
                        yield
                    ipB = hfree.pop(0)
                    pB, pB_r = gp[ipB], gp_r[ipB]
                    yield
                    P.op("pe", lambda e, h=h, pB=pB: e.matmul(pB[:, 0:128], lhsT=lrep[:, h, :], rhs=triu[:], start=True, stop=True),
                         reads=[lrep_r, cons_r], writes=[pB_r])
                    yield
                    P.op("dve", lambda e, H=H, pB=pB: e.tensor_tensor(out=H["argm"][:], in0=pB[:, 0:128], in1=mneg[:], op=ALU.add),
                         reads=[pB_r, cons_r], writes=[H["r"]])
                    yield
                    P.op("act", lambda e, H=H, h=h: e.activation(out=H["dec"][:], in_=H["argm"][:], func=AF.Exp, bias=gts[:, 80 + h:81 + h]),
                         reads=[H["r"], gts_r], writes=[H["r"]])
                    yield
                    P.op("act", lambda e, H=H, pB=pB, r0=r0: e.activation(out=H["eb"][r0:r0 + 64, :], in_=pB[r0:r0 + 64, 0:128], func=AF.Exp),
                         reads=[pB_r], writes=[H["r"]])
                    hfree.append(ipB)
                    yield
                    P.op("dve", lambda e, H=H, r0=r0, hh_=hh_, h=h: e.tensor_tensor(
                        out=qsT[r0:r0 + 64, hh_, :], in0=qT[r0:r0 + 64, hh_, :], in1=H["eb"][r0:r0 + 64, :], op=ALU.mult),
                        reads=[H["r"], qk_r], writes=[qs_r[h]])
                    while not hfree:
                        yield
                    ipS = hfree.pop(0)
                    pS, pS_r = gp[ipS], gp_r[ipS]
                    yield
                    P.op("pe", lambda e, pS=pS, r0=r0, hh_=hh_: e.matmul(
                        pS[:, 0:128], lhsT=kT[r0:r0 + 64, hh_, :], rhs=qT[r0:r0 + 64, hh_, :], start=True, stop=True),
                        reads=[qk_r], writes=[pS_r])
                    yield
                    P.op("dve", lambda e, H=H, pS=pS: e.tensor_tensor(out=H["sc"][:], in0=pS[:, 0:128], in1=H["dec"][:], op=ALU.mult),
                         reads=[pS_r, H["r"]], writes=[H["r"]])
                    hfree.append(ipS)
                    while not hfree:
                        yield
                    ipN = hfree.pop(0)
                    pN, pN_r = gp[ipN], gp_r[ipN]

                    def mmn(e, pN=pN, H=H, h=h, r0=r0, hh_=hh_):
                        e.matmul(pN[:, 0:128], lhsT=Cbf[r0:r0 + 64, hh_, 0:128], rhs=qsT[r0:r0 + 64, hh_, :], start=True, stop=False)
                        e.matmul(pN[:, 0:128], lhsT=vaug[:, h, 0:128], rhs=H["sc"][:], start=False, stop=True)
                        e.matmul(pN[:, 128:256], lhsT=nrep[r0:r0 + 64, hh_, :], rhs=qsT[r0:r0 + 64, hh_, :], start=True, stop=False)
                        return e.matmul(pN[:, 128:256], lhsT=ones_b[:], rhs=H["sc"][:], start=False, stop=True)
                    yield
                    P.op("pe", mmn, reads=[Cbf_r, qs_r[h], vaug_r, H["r"], cons_r], writes=[pN_r])
                    yield
                    P.op("dve", lambda e, H=H, pN=pN: e.tensor_scalar(out=H["dn"][:], in0=pN[:, 128:256], scalar1=1.0, scalar2=None,
                                                                      op0=ALU.max),
                         reads=[pN_r], writes=[H["r"]])
                    yield
                    P.op("dve", lambda e, H=H, pN=pN: e.scalar_tensor_tensor(out=H["dn"][:], in0=pN[:, 128:256], scalar=-1.0, in1=H["dn"][:],
                                                                             op0=ALU.mult, op1=ALU.max),
                         reads=[pN_r, H["r"]], writes=[H["r"]])
                    yield
                    P.op("dve", lambda e, H=H: e.reciprocal(out=H["dn"][:], in_=H["dn"][:]), reads=[H["r"]], writes=[H["r"]])
                    yield
                    P.op("dve", lambda e, H=H, pN=pN: e.tensor_tensor(out=H["hh"][:], in0=pN[:, 0:128], in1=H["dn"][:], op=ALU.mult),
                         reads=[pN_r, H["r"]], writes=[H["r"]])
                    hfree.append(ipN)
                    yield
                    P.op("pool", lambda e, H=H: e.tensor_tensor(out=H["sq"][:], in0=H["hh"][:], in1=H["hh"][:], op=ALU.mult),
                         reads=[H["r"]], writes=[H["r"]])
                    while not hfree:
                        yield
                    ipR = hfree.pop(0)
                    pR, pR_r = gp[ipR], gp_r[ipR]
                    yield
                    P.op("pe", lambda e, H=H, pR=pR: e.matmul(pR[:, 0:128], lhsT=ones_b[:], rhs=H["sq"][:], start=True, stop=True),
                         reads=[H["r"], cons_r], writes=[pR_r])
                    yield
                    P.op("act", lambda e, H=H, pR=pR: e.activation(out=H["rs"][:], in_=pR[:, 0:128], func=AF.Ln, scale=1.0 / 128,
                                                                   bias=epst[:, 0:1]),
                         reads=[pR_r, cons_r], writes=[H["r"]])
                    hfree.append(ipR)
                    yield
                    P.op("act", lambda e, H=H: e.activation(out=H["rs"][:], in_=H["rs"][:], func=AF.Exp, scale=-0.5),
                         reads=[H["r"]], writes=[H["r"]])
                    yield
                    P.op("dve", lambda e, H=H: e.tensor_tensor(out=H["hh"][:], in0=H["hh"][:], in1=H["rs"][:], op=ALU.mult),
                         reads=[H["r"]], writes=[H["r"]])
                    yield
                    P.op("dve", lambda e, H=H, h=h: e.scalar_tensor_tensor(
                        out=gT[:, h, :], in0=H["hh"][:], scalar=smallw[:, 16 + h:17 + h], in1=sgT[:, h, :], op0=ALU.mult, op1=ALU.mult),
                        reads=[H["r"], smallw_r, uT_r], writes=[big_r])
                hfree = list(range(NG))
                for h0 in range(0, 8, 8):
                    gens = [head(h0 + k, hd[k]) for k in range(8)]
                    while gens:
                        for g in list(gens):
                            try:
                                next(g)
                            except StopIteration:
                                gens.remove(g)
                for hh_ in range(4):
                    pC, pC_r = bank()

                    def mmcl(e, pC=pC, hh_=hh_):
                        e.matmul(pC[:, 0:129], lhsT=kw[:, 2 * hh_:2 * hh_ + 2, :].rearrange("p h d -> p (h d)"),
                                 rhs=vaug[:, 2 * hh_, :], start=True, stop=True)
                        return e.matmul(pC[:, 129:258], lhsT=kw[:, 2 * hh_:2 * hh_ + 2, :].rearrange("p h d -> p (h d)"),
                                        rhs=vaug[:, 2 * hh_ + 1, :], start=True, stop=True)
                    P.op("pe", mmcl, reads=[kw_r, vaug_r], writes=[pC_r])
                    for par in range(2):
                        r0 = par * 64
                        h = 2 * hh_ + par
                        P.op("dve", lambda e, pC=pC, hh_=hh_, r0=r0, par=par, h=h: e.scalar_tensor_tensor(
                            out=Cst[r0:r0 + 64, hh_, :], in0=Cst[r0:r0 + 64, hh_, :], scalar=gts[r0:r0 + 64, 88 + h:89 + h],
                            in1=pC[r0:r0 + 64, par * 129:(par + 1) * 129], op0=ALU.mult, op1=ALU.add),
                            reads=[pC_r, gts_r, Cst_r, Cbf_r], writes=[Cst_r])
                P.op("act", lambda e: e.activation(out=Cbf[:].rearrange("p c n -> p (c n)"), in_=Cst[:].rearrange("p c n -> p (c n)"), func=AF.Identity),
                     reads=[Cst_r], writes=[Cbf_r])
                for hh_ in range(4):
                    P.op("pool", lambda e, hh_=hh_: e.tensor_scalar(out=nrep[:, hh_, :], in0=ones_f[:], scalar1=Cst[:, hh_, 128:129],
                                                                    scalar2=None, op0=ALU.mult),
                         reads=[Cst_r, cons_r], writes=[Cbf_r])

                def mm2(e):
                    for n in range(2):
                        for cc in range(8):
                            r = e.matmul(psY[:, n * 512:(n + 1) * 512], lhsT=gT[:, cc, :], rhs=Wout[:, cc, n * 512:(n + 1) * 512],
                                         start=(cc == 0), stop=(cc == 7))
                    return r
                P.op("pe", mm2, reads=[W_r[1], big_r], writes=[psY_r])

            def seq_init(b):
                P.op("dve", lambda e: e.memset(vaug[:], 1.0), writes=[vaug_r])
                P.op("dve", lambda e: e.memset(Cst[:], 0.0), writes=[Cst_r])
                P.op("dve", lambda e: e.memset(Cbf[:], 0.0), writes=[Cbf_r])
                P.op("dve", lambda e: e.memset(nrep[:], 0.0), writes=[Cbf_r])
            drive(si, i, "mix", middle, seq_init)

        for si, (i, which) in enumerate(subs):
            P.barrier(bar_fns, bar_res)
            if which == "ffn":
                ffn(si, i)
            elif i % 2 == 0:
                mlstm(si, i)
            else:
                sgu(si, i)
        P.op("sp", lambda e: e.nop(), reads=dram_r + xt_r)

        with nc.Block() as block:
            P.emit(nc, block, sems)
    return nc


def _consts():
    l = np.arange(128)
    ident = np.eye(128, dtype=np.float32)
    triu = (l[:, None] <= l[None, :]).astype(np.float32)
    mneg = np.where(l[:, None] <= l[None, :], 0.0, -30000.0).astype(np.float32)
    return ident, triu, mneg


def make_in_maps(inputs, S, n_cores):
    f = lambda a: np.ascontiguousarray(np.asarray(a, dtype=np.float32))
    x = f(inputs["x"])
    c = f(inputs["c"])
    ident, triu, mneg = _consts()
    ng = f(inputs["norm_g"])
    shared = {
        "ngT": f(ng.reshape(DEPTH, 4, KD, 128).transpose(3, 0, 1, 2)),
        "norm_g": ng,
        "ada_w": f(inputs["ada_w"]), "ada_b": f(inputs["ada_b"]),
        "ffn_w1": f(inputs["ffn_w1"]), "ffn_w2": f(inputs["ffn_w2"]),
        "a_w_in": f(inputs["a_w_in"]), "a_b_if": f(inputs["a_b_if"]),
        "a_hgT": f(f(inputs["a_hnorm_g"]).reshape(2, 8, 128).transpose(0, 2, 1)),
        "a_w_out": f(inputs["a_w_out"]),
        "b_w_in": f(inputs["b_w_in"]),
        "b_binT": f(f(inputs["b_b_in"])[:, 0:2048].reshape(2, 16, 128).transpose(0, 2, 1)),
        "b_b_in": f(inputs["b_b_in"]),
        "b_ln_g": f(inputs["b_ln_g"]), "b_ln_b": f(inputs["b_ln_b"]),
        "b_wsT": f(f(inputs["b_ws"]).transpose(0, 3, 1, 2)),
        "b_bs": f(f(inputs["b_bs"]).reshape(2, 1, 2048)),
        "b_w_out": f(inputs["b_w_out"]),
        "c_ident": ident, "c_triu": triu, "c_mneg": mneg,
    }
    maps = []
    for ci in range(n_cores):
        m = dict(shared)
        m["x"] = f(x[ci * NB:(ci + 1) * NB, :S].reshape(NB * S, D))
        m["cT"] = f(c[ci * NB:(ci + 1) * NB].reshape(NB, KD, 128).transpose(2, 1, 0))
        maps.append(m)
    return maps


def run(inputs, S, subs, n_cores):
    nc = build_program(S, subs)
    maps = make_in_maps(inputs, S, n_cores)
    res = run_bass_kernel_spmd(nc, maps, core_ids=list(range(n_cores)))
    outs = [r["out"].reshape(NB, S, D) for r in res.results]
    return np.concatenate(outs, axis=0)


def kernel(**inputs):
    out = run(inputs, 2048, ALL_SUBS, 8)
    return out.astype(np.float32)
```
